# Optimizing a Trainium2 kernel written in Bass

```python
import jax, jax.numpy as jnp
from jax import lax
import numpy as np

D_MODEL = 4096
BATCH = 4
SEQ = 2048
DEPTH = 2

D_FF = 11008
EPS = 1e-6

A_HEADS = 32
A_KV_HEADS = 4
A_HEAD_DIM = 64
WINDOW = 128
A_WIDTH = A_HEADS * A_HEAD_DIM
A_KV_WIDTH = A_KV_HEADS * A_HEAD_DIM

B_GROUPS = 16
B_GROUP_DIM = 128
B_CHUNK = 128
B_WIDTH = B_GROUPS * B_GROUP_DIM

AB_IN = A_WIDTH + 2 * A_KV_WIDTH + 2 * B_WIDTH
AB_OUT = A_WIDTH + B_WIDTH

C_EXPAND = 128
C_HEADS = D_MODEL // C_EXPAND
C_KEY_DIM = C_EXPAND
C_VAL_DIM = D_MODEL // C_HEADS
C_WIDTH = C_HEADS * C_KEY_DIM
C_VWIDTH = C_HEADS * C_VAL_DIM
C_CHUNK = 32
C_IN = 2 * C_WIDTH + 2 * C_VWIDTH

N_AB = (DEPTH + 1) // 2
N_C = DEPTH // 2

kernel_name = "hybrid_swa_gmlp_hgrn2_macaron"


def rms_norm(x, g):
    xf = x.astype(jnp.float32)
    y = xf * lax.rsqrt(jnp.mean(xf * xf, axis=-1, keepdims=True) + EPS)
    return (y * g.astype(jnp.float32)).astype(x.dtype)


def swiglu(h, w1, w3, w2):
    return (jax.nn.silu(h @ w1) * (h @ w3)) @ w2


def sliding_window_attention(q, k, v, q_gain, k_gain, sinks):
    b, s, _, hd = q.shape
    nb = s // WINDOW
    grp = A_HEADS // A_KV_HEADS
    q = rms_norm(q, q_gain)
    k = rms_norm(k, k_gain)
    qb = q.reshape(b, nb, WINDOW, A_KV_HEADS, grp, hd)
    kb = k.reshape(b, nb, WINDOW, A_KV_HEADS, hd)
    vb = v.reshape(b, nb, WINDOW, A_KV_HEADS, hd)
    prev = lambda t: jnp.concatenate([jnp.zeros_like(t[:, :1]), t[:, :-1]], axis=1)
    kk = jnp.concatenate([prev(kb), kb], axis=2)
    vv = jnp.concatenate([prev(vb), vb], axis=2)
    scores = jnp.einsum('bnqhgd,bnkhd->bhgnqk', qb, kk).astype(jnp.float32) * (hd ** -0.5)
    qi = jnp.arange(WINDOW)[:, None] + WINDOW
    kj = jnp.arange(2 * WINDOW)[None, :]
    dist = qi - kj
    band = (dist >= 0) & (dist < WINDOW)
    exists = (jnp.arange(nb)[:, None, None] * WINDOW + kj[None] - WINDOW) >= 0
    mask = band[None] & exists
    scores = jnp.where(mask, scores, -jnp.inf)
    sink = jnp.broadcast_to(sinks.astype(jnp.float32).reshape(A_KV_HEADS, grp, 1, 1, 1),
                            scores.shape[:-1] + (1,))
    probs = jax.nn.softmax(jnp.concatenate([scores, sink], axis=-1), axis=-1)[..., :-1]
    out = jnp.einsum('bhgnqk,bnkhd->bnqhgd', probs.astype(vv.dtype), vv)
    return out.reshape(b, s, A_WIDTH)


def chunked_spatial_gating(u, v, ln_g, ln_b, w_s, b_s):
    b, s, _ = u.shape
    nc = s // B_CHUNK
    vg = v.reshape(b, nc, B_CHUNK, B_GROUPS, B_GROUP_DIM).astype(jnp.float32)
    mu = jnp.mean(vg, axis=-1, keepdims=True)
    var = jnp.mean(jnp.square(vg - mu), axis=-1, keepdims=True)
    vn = ((vg - mu) * lax.rsqrt(var + EPS) * ln_g.astype(jnp.float32).reshape(B_GROUPS, B_GROUP_DIM)
          + ln_b.astype(jnp.float32).reshape(B_GROUPS, B_GROUP_DIM)).astype(u.dtype)
    causal = jnp.tril(jnp.ones((B_CHUNK, B_CHUNK), dtype=bool))
    w = jnp.where(causal[None], w_s, jnp.zeros_like(w_s))
    sg = jnp.einsum('gts,bnsgc->bntgc', w, vn) + b_s.T[None, None, :, :, None]
    ug = u.reshape(b, nc, B_CHUNK, B_GROUPS, B_GROUP_DIM)
    return (ug * sg).reshape(b, s, B_WIDTH)


def hgrn2_recurrence(q, k, v, log_f):
    b, s, h, dk = q.shape
    dv = v.shape[-1]
    nc = s // C_CHUNK
    def to_chunks(t):
        return t.astype(jnp.float32).reshape(b, nc, C_CHUNK, h, t.shape[-1]).transpose(1, 0, 3, 2, 4)
    qc, kc, vc, gc = to_chunks(q), to_chunks(k), to_chunks(v), to_chunks(log_f)
    causal = jnp.tril(jnp.ones((C_CHUNK, C_CHUNK), dtype=bool))[:, :, None]

    def step(state, inp):
        qt, kt, vt, gt = inp
        cg = jnp.cumsum(gt, axis=2)
        o_inter = jnp.einsum('bhtd,bhde->bhte', qt * jnp.exp(cg), state)
        diff = cg[:, :, :, None, :] - cg[:, :, None, :, :]
        decay = jnp.exp(jnp.where(causal, diff, -jnp.inf))
        att = jnp.einsum('bhtd,bhsd,bhtsd->bhts', qt, kt, decay)
        o_intra = jnp.einsum('bhts,bhse->bhte', att, vt)
        last = cg[:, :, -1:, :]
        state = (jnp.exp(last[:, :, 0, :])[..., None] * state
                 + jnp.einsum('bhsd,bhse->bhde', kt * jnp.exp(last - cg), vt))
        return state, o_inter + o_intra

    s0 = jnp.zeros((b, h, dk, dv), jnp.float32)
    _, o = lax.scan(step, s0, (qc, kc, vc, gc))
    return o.transpose(1, 0, 3, 2, 4).reshape(b, s, h, dv).astype(q.dtype)


def ab_mixer(h, w_in, q_gain, k_gain, sinks, v_ln_g, v_ln_b, w_s, b_s, w_out):
    b, s, _ = h.shape
    proj = h @ w_in
    i1 = A_WIDTH
    i2 = i1 + A_KV_WIDTH
    i3 = i2 + A_KV_WIDTH
    i4 = i3 + B_WIDTH
    q, k, v, ub, vb = jnp.split(proj, [i1, i2, i3, i4], axis=-1)
    out_a = sliding_window_attention(q.reshape(b, s, A_HEADS, A_HEAD_DIM),
                                     k.reshape(b, s, A_KV_HEADS, A_HEAD_DIM),
                                     v.reshape(b, s, A_KV_HEADS, A_HEAD_DIM),
                                     q_gain, k_gain, sinks)
    out_b = chunked_spatial_gating(jax.nn.gelu(ub, approximate=False), jax.nn.gelu(vb, approximate=False),
                                   v_ln_g, v_ln_b, w_s, b_s)
    return jnp.concatenate([out_a, out_b], axis=-1) @ w_out


def hgrn2_mixer(h, w_in, lb, o_gain, w_out):
    b, s, _ = h.shape
    proj = h @ w_in
    q, fz, inp, gate = jnp.split(proj, [C_WIDTH, 2 * C_WIDTH, 2 * C_WIDTH + C_VWIDTH], axis=-1)
    zf = fz.astype(jnp.float32)
    lbf = lb.astype(jnp.float32)
    log_f = jnp.logaddexp(jnp.log(lbf), jnp.log1p(-lbf) + jax.nn.log_sigmoid(zf))
    k = (1.0 - lbf) * jax.nn.sigmoid(-zf)
    o = hgrn2_recurrence(q.reshape(b, s, C_HEADS, C_KEY_DIM),
                         k.reshape(b, s, C_HEADS, C_KEY_DIM).astype(q.dtype),
                         inp.reshape(b, s, C_HEADS, C_VAL_DIM),
                         log_f.reshape(b, s, C_HEADS, C_KEY_DIM))
    o = rms_norm(o, o_gain).reshape(b, s, C_VWIDTH) * jax.nn.silu(gate)
    return o @ w_out


def setup_inputs(seed: int = 0) -> dict:
    key = jax.random.key(seed)
    ks = jax.random.split(key, 24)
    f32 = jnp.float32
    def w(k, shape, fan_in):
        return jax.random.normal(k, shape, f32) * (fan_in ** -0.5)
    def gain(k, shape):
        return 1.0 + 0.05 * jax.random.normal(k, shape, f32)
    return {
        "x": jax.random.normal(ks[0], (BATCH, SEQ, D_MODEL), f32),
        "ffn1_norm": gain(ks[1], (DEPTH, D_MODEL)),
        "ffn1_w1": w(ks[2], (DEPTH, D_MODEL, D_FF), D_MODEL),
        "ffn1_w3": w(ks[3], (DEPTH, D_MODEL, D_FF), D_MODEL),
        "ffn1_w2": w(ks[4], (DEPTH, D_FF, D_MODEL), D_FF),
        "mix_norm": gain(ks[5], (DEPTH, D_MODEL)),
        "ffn2_norm": gain(ks[6], (DEPTH, D_MODEL)),
        "ffn2_w1": w(ks[7], (DEPTH, D_MODEL, D_FF), D_MODEL),
        "ffn2_w3": w(ks[8], (DEPTH, D_MODEL, D_FF), D_MODEL),
        "ffn2_w2": w(ks[9], (DEPTH, D_FF, D_MODEL), D_FF),
        "ab_w_in": w(ks[10], (N_AB, D_MODEL, AB_IN), D_MODEL),
        "ab_q_norm": gain(ks[11], (N_AB, A_HEAD_DIM)),
        "ab_k_norm": gain(ks[12], (N_AB, A_HEAD_DIM)),
        "ab_sinks": 0.5 * jax.random.normal(ks[13], (N_AB, A_HEADS), f32),
        "ab_v_ln_g": gain(ks[14], (N_AB, B_WIDTH)),
        "ab_v_ln_b": 0.02 * jax.random.normal(ks[15], (N_AB, B_WIDTH), f32),
        "ab_w_s": w(ks[16], (N_AB, B_GROUPS, B_CHUNK, B_CHUNK), B_CHUNK),
        "ab_b_s": gain(ks[17], (N_AB, B_GROUPS, B_CHUNK)),
        "ab_w_out": w(ks[18], (N_AB, AB_OUT, D_MODEL), AB_OUT),
        "c_w_in": w(ks[19], (N_C, D_MODEL, C_IN), D_MODEL),
        "c_lb_logits": jax.random.normal(ks[20], (DEPTH, C_WIDTH), f32),
        "c_o_norm": gain(ks[21], (N_C, C_VAL_DIM)),
        "c_w_out": w(ks[22], (N_C, C_VWIDTH, D_MODEL), C_VWIDTH),
    }


def reference(x, ffn1_norm, ffn1_w1, ffn1_w3, ffn1_w2, mix_norm, ffn2_norm, ffn2_w1, ffn2_w3, ffn2_w2,
              ab_w_in, ab_q_norm, ab_k_norm, ab_sinks, ab_v_ln_g, ab_v_ln_b, ab_w_s, ab_b_s, ab_w_out,
              c_w_in, c_lb_logits, c_o_norm, c_w_out):
    p = jax.nn.softmax(c_lb_logits.astype(jnp.float32), axis=0)
    lower_bounds = jnp.cumsum(p, axis=0) - p[0:1]
    for l in range(DEPTH):
        x = x + 0.5 * swiglu(rms_norm(x, ffn1_norm[l]), ffn1_w1[l], ffn1_w3[l], ffn1_w2[l])
        h = rms_norm(x, mix_norm[l])
        if l % 2 == 0:
            j = l // 2
            x = x + ab_mixer(h, ab_w_in[j], ab_q_norm[j], ab_k_norm[j], ab_sinks[j],
                             ab_v_ln_g[j], ab_v_ln_b[j], ab_w_s[j], ab_b_s[j], ab_w_out[j])
        else:
            j = l // 2
            x = x + hgrn2_mixer(h, c_w_in[j], lower_bounds[l], c_o_norm[j], c_w_out[j])
        x = x + 0.5 * swiglu(rms_norm(x, ffn2_norm[l]), ffn2_w1[l], ffn2_w3[l], ffn2_w2[l])
    return x
```

```python
import numpy as np
import concourse.bass as bass
import concourse.mybir as mybir
from concourse.bass_utils import run_bass_kernel_spmd

F32 = mybir.dt.float32
BF16 = mybir.dt.bfloat16
AF = mybir.ActivationFunctionType
ALU = mybir.AluOpType
AX = mybir.AxisListType

D = 4096
FF = 11008
KT = D // 128
EPS = 1e-6
ENGS = ("pe", "act", "dve", "pool", "sp")


class Sem:
    uid = 0

    def __init__(self, nc, name):
        self.h = nc.alloc_semaphore(name)
        self.n = 0
        Sem.uid += 1
        self.uid = Sem.uid


class Prog:
    def __init__(self, nc):
        self.nc = nc
        self.ops = {e: [] for e in ENGS}
        self.waited = {}
        self.nsem = 0
        self._shared = {}

    def sem(self, name):
        self.nsem += 1
        return Sem(self.nc, f"{name}_{self.nsem}")

    def shared(self, name):
        if name not in self._shared:
            self._shared[name] = self.sem(name)
        return self._shared[name]

    def op(self, eng, fn, waits=(), inc=None, k=1):
        ws = []
        for t in waits:
            if t is None:
                continue
            s, v = t
            key = (eng, s.uid)
            if self.waited.get(key, 0) >= v:
                continue
            self.waited[key] = v
            ws.append((s.h, v))
        tick = None
        if inc is not None:
            inc.n += k
            tick = (inc, inc.n)
        self.ops[eng].append((fn, ws, (inc.h, k) if inc is not None else None))
        return tick

    def dma(self, eng, out, in_, waits=(), inc=None):
        nc = self.nc
        q = {"sp": nc.sync, "pool": nc.gpsimd, "act": nc.scalar}[eng]
        return self.op(eng, lambda: q.dma_start(out=out, in_=in_), waits, inc, 16)

    def wait(self, eng, tickets):
        self.op(eng, None, waits=tickets)

    def emit(self):
        nc = self.nc

        def run(engine, lst, attach):
            for fn, ws, inc in lst:
                if fn is None or not attach or not ws:
                    for h, v in ws:
                        engine.wait_ge(h, v)
                    ws = []
                else:
                    for h, v in ws[:-1]:
                        engine.wait_ge(h, v)
                    ws = ws[-1:]
                if fn is None:
                    continue
                ins = fn()
                for h, v in ws:
                    ins._wait_ge(h, v)
                if inc is not None:
                    ins.then_inc(inc[0], inc[1])

        with nc.Block() as block:
            @block.tensor
            def _(e):
                run(e, self.ops["pe"], False)

            @block.scalar
            def _(e):
                run(e, self.ops["act"], True)

            @block.vector
            def _(e):
                run(e, self.ops["dve"], True)

            @block.gpsimd
            def _(e):
                run(e, self.ops["pool"], True)

            @block.sync
            def _(e):
                run(e, self.ops["sp"], True)


def ntiles_of(T):
    out = []
    s = 0
    while s < T:
        n = min(512, T - s)
        out.append((s, n))
        s += n
    return out


class PsumSets:
    def __init__(self, P, ps):
        self.P = P
        self.ps = ps
        self.free = [None, None]
        self.u = 0

    def next(self):
        s = self.u % 2
        self.u += 1
        return s, self.free[s]

    def release(self, s, ticket):
        self.free[s] = ticket


def load_colvec(P, ctx, vec_ap, n, ident, pe_done, dve_done, pss, name, sp_sem, waits=()):
    nc = P.nc
    rows = ctx.enter_context(nc.sbuf_tensor(name + "_r", [n, 128], F32))
    colv = ctx.enter_context(nc.sbuf_tensor(name + "_c", [128, n], F32))
    t_ld = P.dma("sp", rows[:], vec_ap.rearrange("(k p) -> k p", p=128), waits=list(waits), inc=sp_sem)
    s, fr = pss.next()
    t_pe = P.op("pe", lambda: nc.tensor.transpose(out=pss.ps[:, 4 * s, 0:n], in_=rows[:], identity=ident[0:n, 0:n]),
                waits=[t_ld, fr], inc=pe_done)
    t_cp = P.op("dve", lambda: nc.vector.tensor_copy(out=colv[:], in_=pss.ps[:, 4 * s, 0:n]), waits=[t_pe], inc=dve_done)
    pss.release(s, t_cp)
    return colv, t_cp


def rmsnorm_T(P, ctx, pss, x_src, T, gain_ap, ident, xnT, x_ready, name):
    nc = P.nc
    ntt = T // 128
    pe_done = P.shared("norm_pe")
    dve_done = P.shared("norm_dve")
    act_done = P.shared("norm_act")
    ldsem = [P.shared("norm_ld0"), P.shared("norm_ld1")]
    gsem = P.shared("norm_g")
    from contextlib import ExitStack
    with ExitStack() as sctx:
        gcol, t_g = load_colvec(P, sctx, gain_ap, KT, ident, pe_done, dve_done, pss, name + "gv", gsem, waits=x_ready)
        xt = sctx.enter_context(nc.sbuf_tensor(name + "xt", [128, 2, D], F32))
        junk = sctx.enter_context(nc.sbuf_tensor(name + "junk", [128, D], BF16))
        ss = sctx.enter_context(nc.sbuf_tensor(name + "ss", [128, ntt], F32))
        rstd = sctx.enter_context(nc.sbuf_tensor(name + "rstd", [128, ntt], F32))
        epsc = sctx.enter_context(nc.sbuf_tensor(name + "epsc", [128, 1], F32))
        t_eps = P.op("dve", lambda: nc.vector.memset(epsc[:], EPS), waits=list(x_ready), inc=dve_done)
        xt_free = [None, None]
        junk_free = None
        last = None
        for tt in range(ntt):
            sl = tt % 2
            t_ld = P.dma("sp", xt[:, sl, :], x_src[tt * 128:(tt + 1) * 128, :], waits=list(x_ready) + [xt_free[sl]], inc=ldsem[sl])
            t_sq = P.op("act", lambda sl=sl, tt=tt: nc.scalar.activation(out=junk[:], in_=xt[:, sl, :], func=AF.Square,
                                                                         accum_out=ss[:, tt:tt + 1]),
                        waits=[t_ld, junk_free], inc=act_done)
            junk_free = t_sq
            t_r1 = P.op("act", lambda tt=tt: nc.scalar.activation(out=rstd[:, tt:tt + 1], in_=ss[:, tt:tt + 1], func=AF.Sqrt,
                                                                  scale=1.0 / D, bias=epsc[:, 0:1]),
                        waits=[t_sq, t_eps], inc=act_done)
            t_r2 = P.op("dve", lambda tt=tt: nc.vector.reciprocal(out=rstd[:, tt:tt + 1], in_=rstd[:, tt:tt + 1]),
                        waits=[t_r1], inc=dve_done)
            t_sc = P.op("act", lambda sl=sl, tt=tt: nc.scalar.activation(out=xt[:, sl, :], in_=xt[:, sl, :], func=AF.Copy,
                                                                         scale=rstd[:, tt:tt + 1]),
                        waits=[t_r2], inc=act_done)
            for h in range(2):
                s, fr = pss.next()
                t_pe = None
                for j in range(16):
                    k = h * 16 + j
                    t_pe = P.op("pe", lambda sl=sl, k=k, s=s, j=j: nc.tensor.transpose(
                        out=pss.ps[:, 4 * s + j // 4, (j % 4) * 128:(j % 4 + 1) * 128],
                        in_=xt[:, sl, k * 128:(k + 1) * 128], identity=ident[:, :]),
                        waits=[t_sc, fr], inc=pe_done if j == 15 else None)
                t_ev = None
                for b in range(4):
                    k0 = h * 16 + b * 4
                    t_ev = P.op("dve", lambda s=s, b=b, k0=k0, tt=tt: nc.vector.tensor_tensor(
                        out=xnT[:, k0:k0 + 4, tt * 128:(tt + 1) * 128],
                        in0=pss.ps[:, 4 * s + b, :].rearrange("p (j t) -> p j t", j=4),
                        in1=gcol[:, k0:k0 + 4].unsqueeze(2).to_broadcast([128, 4, 128]), op=ALU.mult),
                        waits=[t_pe, t_g], inc=dve_done)
                pss.release(s, t_ev)
                last = t_ev
            xt_free[sl] = t_pe
    return last


class WeightStream:
    def __init__(self, P, name, tensor, nslots, sname=None):
        self.P = P
        self.t = tensor
        self.n = nslots
        self.ld = [P.shared(f"{sname or name}ld{i}") for i in range(nslots)]
        self.free = [None] * nslots
        self.i = 0
        self.extra = []

    def load(self, dst_fn, src):
        sl = self.i % self.n
        ex = self.extra if self.i < self.n else []
        self.i += 1
        t = self.P.dma("pool", dst_fn(self.t, sl), src, waits=[self.free[sl]] + list(ex), inc=self.ld[sl])
        return sl, t

    def release(self, sl, ticket):
        self.free[sl] = ticket


class Ctx:
    pass


def make_ctx(P, stack, ident_ap, GMAX=14, TMAX=1152):
    nc = P.nc
    C = Ctx()
    C.GMAX = GMAX
    C.ps = stack.enter_context(nc.psum_tensor("ps", [128, 8, 512], F32))
    C.pss = PsumSets(P, C.ps)
    C.ident = stack.enter_context(nc.sbuf_tensor("ident_sb", [128, 128], F32))
    C.xnT = stack.enter_context(nc.sbuf_tensor("xnT", [128, KT, TMAX], BF16))
    C.w13 = stack.enter_context(nc.sbuf_tensor("w13", [128, 3, KT, 256], BF16))
    C.ws13 = WeightStream(P, "ws13", C.w13, 3)
    isem = P.sem("identld")
    t_i = P.dma("sp", C.ident[:], ident_ap, inc=isem)
    C.identb = stack.enter_context(nc.sbuf_tensor("identb_sb", [128, 128], BF16))
    C.ones128 = stack.enter_context(nc.sbuf_tensor("ones128_sb", [128, 128], F32))
    C.epsc = stack.enter_context(nc.sbuf_tensor("epsc_sb", [128, 1], F32))
    csem = P.sem("ctxdve")
    P.op("dve", lambda: nc.vector.memset(C.ones128[:], 1.0))
    P.op("dve", lambda: nc.vector.memset(C.epsc[:], EPS))
    C.t_ident = P.op("dve", lambda: nc.vector.tensor_copy(out=C.identb[:], in_=C.ident[:]), waits=[t_i], inc=csem)
    return C


def ffn_stage(P, C, x_src, x_dst, T, gain_ap, W1, W3, W2, x_ready, name, nch=FF // 128):
    nc = P.nc
    from contextlib import ExitStack
    pss = C.pss
    nts = ntiles_of(T)
    ntt = T // 128
    NP = nch // 2
    assert nch % 2 == 0
    gp_max = C.GMAX // 2 if T <= 1024 else (C.GMAX - 2) // 2
    ngrp = -(-NP // gp_max)
    base = NP // ngrp
    gsz = [base + (1 if i < NP - base * ngrp else 0) for i in range(ngrp)]
    tsets = [list(range(i, min(i + 4, ntt))) for i in range(0, ntt, 4)]
    xnT = C.xnT
    w13 = C.w13

    with ExitStack() as ctx:
        pe_u = P.sem(name + "peu")
        act_d = P.sem(name + "actd")
        dve_d = P.sem(name + "dved")
        xs_ld = [P.shared(f"xs_ld{i}") for i in range(3)]
        xs_st = [P.shared(f"xs_st{i}") for i in range(3)]

        gmax = 2 * max(gsz)
        w2b = ctx.enter_context(nc.sbuf_tensor(name + "w2b", [128, 2, gmax, 512], BF16))
        ws2 = WeightStream(P, name + "ws2", w2b, 2, sname="ws2")
        ws2.extra = list(x_ready)
        t_norm = rmsnorm_T(P, ctx, pss, x_src, T, gain_ap, C.ident, xnT, x_ready, name + "n")

        gT = ctx.enter_context(nc.sbuf_tensor(name + "gT", [128, gmax, T], BF16))
        tmp = ctx.enter_context(nc.sbuf_tensor(name + "tmp", [128, 2, T], BF16))
        xs = ctx.enter_context(nc.sbuf_tensor(name + "xs", [128, 3, 4, 512], F32))

        st_tick = {}
        xs_free = [None, None, None]
        xs_state = {"i": 0}

        def issue_xload(g, n, si):
            ts = tsets[si]
            xi = xs_state["i"]
            xs_state["i"] += 1
            xsl = xi % 3
            r0 = ts[0] * 128
            nr = len(ts)
            src_t = x_src if g == 0 else x_dst
            src = src_t[r0:r0 + nr * 128, n * 512:(n + 1) * 512].rearrange("(j p) c -> p j c", p=128)
            t_ld = P.dma("sp", xs[:, xsl, 0:nr, :], src,
                         waits=[xs_free[xsl], st_tick.get((si, n))] + (list(x_ready) + [t_norm] if g == 0 else []), inc=xs_ld[xsl])
            return xsl, t_ld

        tmp_free = [None, None]
        final = []
        prefetched = {}
        p0 = 0
        for g in range(ngrp):
            np_ = gsz[g]
            pp = p0
            p0 += np_
            G = 2 * np_
            t_gT = None
            for pi in range(np_):
                p = pp + pi
                wl = []
                for which in range(2):
                    if (p, which) in prefetched:
                        wl.append(prefetched.pop((p, which)))
                    else:
                        wl.append(C.ws13.load(lambda t, sl: t[:, sl], (W1 if which == 0 else W3)[p]))
                for ci in range(2):
                    gi = 2 * pi + ci
                    tsl = gi % 2
                    t_act = None
                    for which in range(2):
                        slw, t_w = wl[which]
                        s, fr = pss.next()
                        t_pe = None
                        for k in range(KT):
                            for bi, (n0, nn) in enumerate(nts):
                                lastmm = (k == KT - 1 and bi == len(nts) - 1)
                                t_pe = P.op("pe", lambda slw=slw, k=k, ci=ci, s=s, bi=bi, n0=n0, nn=nn: nc.tensor.matmul(
                                    pss.ps[:, 4 * s + bi, 0:nn], lhsT=w13[:, slw, k, ci * 128:(ci + 1) * 128],
                                    rhs=xnT[:, k, n0:n0 + nn], start=(k == 0), stop=(k == KT - 1)),
                                    waits=[t_w, fr, t_norm], inc=pe_u if lastmm else None)
                        if ci == 1:
                            C.ws13.release(slw, t_pe)
                        psv = pss.ps[:, 4 * s:4 * s + 4, :].rearrange("p b n -> p (b n)")[:, 0:T]
                        if which == 0:
                            t_act = P.op("act", lambda psv=psv, tsl=tsl: nc.scalar.activation(out=tmp[:, tsl, :], in_=psv, func=AF.Silu),
                                         waits=[t_pe, tmp_free[tsl]], inc=act_d)
                            pss.release(s, t_act)
                        else:
                            t_d = P.op("dve", lambda psv=psv, tsl=tsl, gi=gi: nc.vector.tensor_tensor(
                                out=gT[:, gi, :], in0=psv, in1=tmp[:, tsl, :], op=ALU.mult),
                                waits=[t_pe, t_act], inc=dve_d)
                            pss.release(s, t_d)
                            tmp_free[tsl] = t_d
                            t_gT = t_d
            units = [(n, si) for n in range(8) for si in range(len(tsets))]
            pre = issue_xload(g, units[0][0], units[0][1])
            wcur = None
            for ui, (n, si) in enumerate(units):
                ts = tsets[si]
                nr = len(ts)
                if si == 0:
                    src = W2[n][:, 2 * pp:2 * pp + G, :]
                    wcur = ws2.load(lambda t, sl, G=G: t[:, sl, 0:G, :], src)
                    if n == 2 and g + 1 < ngrp:
                        pn = pp + np_
                        for which in range(2):
                            prefetched[(pn, which)] = C.ws13.load(lambda t, sl: t[:, sl], (W1 if which == 0 else W3)[pn])
                        if gsz[g + 1] > 1:
                            prefetched[(pn + 1, 0)] = C.ws13.load(lambda t, sl: t[:, sl], W1[pn + 1])
                slw, t_w = wcur
                xsl, t_ld = pre
                if ui + 1 < len(units):
                    pre = issue_xload(g, units[ui + 1][0], units[ui + 1][1])
                s, fr = pss.next()
                t_pe = None
                for c in range(G):
                    for j, tt in enumerate(ts):
                        lastmm = (c == G - 1 and j == nr - 1)
                        t_pe = P.op("pe", lambda c=c, tt=tt, s=s, j=j, slw=slw, G=G: nc.tensor.matmul(
                            pss.ps[:, 4 * s + j, :], lhsT=gT[:, c, tt * 128:(tt + 1) * 128], rhs=w2b[:, slw, c, :],
                            start=(c == 0), stop=(c == G - 1)),
                            waits=[t_w, fr, t_gT], inc=pe_u if lastmm else None)
                if si == len(tsets) - 1:
                    ws2.release(slw, t_pe)
                t_ev = None
                for j in range(nr):
                    t_ev = P.op("dve", lambda s=s, j=j, xsl=xsl: nc.vector.scalar_tensor_tensor(
                        out=xs[:, xsl, j, :], in0=pss.ps[:, 4 * s + j, :], scalar=0.5, in1=xs[:, xsl, j, :],
                        op0=ALU.mult, op1=ALU.add),
                        waits=[t_pe, t_ld], inc=dve_d)
                pss.release(s, t_ev)
                r0 = ts[0] * 128
                dst = x_dst[r0:r0 + nr * 128, n * 512:(n + 1) * 512].rearrange("(j p) c -> p j c", p=128)
                t_st = P.dma("sp", dst, xs[:, xsl, 0:nr, :], waits=[t_ev], inc=xs_st[xsl])
                xs_free[xsl] = t_st
                st_tick[(si, n)] = t_st
                if g == ngrp - 1:
                    final.append(t_st)
    return final


def accum_rows(P, C, name, actT, G, load_w, x, row0, scale, first_waits, st_tick, dve_d, pe_u, sctx):
    nc = P.nc
    pss = C.pss
    xs = sctx.enter_context(nc.sbuf_tensor(name + "xs", [128, 3, 4, 512], F32))
    xs_ld = [P.shared(f"xs_ld{i}") for i in range(3)]
    xs_st = [P.shared(f"xs_st{i}") for i in range(3)]
    xs_free = [None, None, None]
    tsets = [[0, 1, 2, 3], [4, 5, 6, 7]]
    units = [(n, si) for n in range(8) for si in range(2)]
    state = {"i": 0}

    def xload(n, si):
        xi = state["i"]
        state["i"] += 1
        xsl = xi % 3
        r0 = row0 + tsets[si][0] * 128
        src = x[r0:r0 + 512, n * 512:(n + 1) * 512].rearrange("(j p) c -> p j c", p=128)
        t = P.dma("sp", xs[:, xsl, :, :], src, waits=[xs_free[xsl], st_tick.get((si, n))] + list(first_waits), inc=xs_ld[xsl])
        return xsl, t

    pre = xload(*units[0])
    final = []
    wcur = None
    for ui, (n, si) in enumerate(units):
        ts = tsets[si]
        if si == 0:
            wcur = load_w(n)
        wfn, t_w, rel = wcur
        xsl, t_ld = pre
        if ui + 1 < len(units):
            pre = xload(*units[ui + 1])
        s, fr = pss.next()
        t_pe = None
        for c in range(G):
            for j, tt in enumerate(ts):
                lastmm = (c == G - 1 and j == 3)
                t_pe = P.op("pe", lambda c=c, tt=tt, s=s, j=j, wfn=wfn: nc.tensor.matmul(
                    pss.ps[:, 4 * s + j, :], lhsT=actT[:, c, tt * 128:(tt + 1) * 128], rhs=wfn(c),
                    start=(c == 0), stop=(c == G - 1)),
                    waits=[t_w, fr] + list(first_waits), inc=pe_u if lastmm else None)
        if si == 1:
            rel(t_pe)
        t_ev = None
        for j in range(4):
            t_ev = P.op("dve", lambda s=s, j=j, xsl=xsl: nc.vector.scalar_tensor_tensor(
                out=xs[:, xsl, j, :], in0=pss.ps[:, 4 * s + j, :], scalar=scale, in1=xs[:, xsl, j, :],
                op0=ALU.mult, op1=ALU.add),
                waits=[t_pe, t_ld], inc=dve_d)
        pss.release(s, t_ev)
        r0 = row0 + ts[0] * 128
        dst = x[r0:r0 + 512, n * 512:(n + 1) * 512].rearrange("(j p) c -> p j c", p=128)
        t_st = P.dma("sp", dst, xs[:, xsl, :, :], waits=[t_ev], inc=xs_st[xsl])
        xs_free[xsl] = t_st
        st_tick[(si, n)] = t_st
        final.append(t_st)
    return final


def ab_stage(P, C, x, aps, x_ready, name, halo, groups, dbg=None):
    from contextlib import ExitStack
    nc = P.nc
    pss = C.pss
    xnT = C.xnT
    w13 = C.w13
    T, TO = 1152, 1024
    Winr = aps["ab_w_in"]
    Wout = aps["ab_w_out"]
    nts_o = ntiles_of(TO)

    with ExitStack() as ctx:
        pe_u = P.sem(name + "peu")
        act_d = P.sem(name + "actd")
        dve_d = P.sem(name + "dved")
        csem = P.sem(name + "csem")
        t_norm = rmsnorm_T(P, ctx, pss, x, TO, aps["mix_norm"], C.ident, xnT, x_ready, name + "n")
        W0 = [t_norm]

        def sb(nm, shape, dt):
            return ctx.enter_context(nc.sbuf_tensor(name + nm, shape, dt))

        qg = sb("qg", [64, 1], F32)
        kg = sb("kg", [64, 1], F32)
        esink = sb("esink", [128, 16], F32)
        mk_cur = sb("mkc", [128, 128], F32)
        mk_prev = sb("mkp", [128, 128], F32)
        mk_prev0 = sb("mkp0", [128, 128], F32)
        ones64 = sb("ones64", [64, 64], F32)
        onespad = sb("onespad", [128, 192], BF16)
        epsc = sb("epsc", [128, 1], F32)
        outT = sb("outT", [128, 16, TO], BF16)

        t_c = None
        for dst, srcap in ((qg[:], aps["qg"]), (kg[:], aps["kg"]), (esink[:], aps["sinks_l"]), (mk_cur[:], aps["mk_cur"]),
                           (mk_prev[:], aps["mk_prev"]), (mk_prev0[:], aps["mk_prev0"])):
            t_c = P.dma("sp", dst, srcap, waits=W0, inc=csem)
        t_es = P.op("act", lambda: nc.scalar.activation(out=esink[:], in_=esink[:], func=AF.Exp), waits=[t_c], inc=act_d)
        P.op("dve", lambda: nc.vector.memset(ones64[:], 1.0), waits=W0)
        P.op("dve", lambda: nc.vector.memset(onespad[:], 0.0), waits=W0)
        P.op("dve", lambda: nc.vector.memset(onespad[:, 64:128], 1.0))
        t_ms = P.op("dve", lambda: nc.vector.memset(epsc[:], EPS), inc=dve_d)

        def slab_load(j):
            return C.ws13.load(lambda t, sl: t[:, sl], Winr[j])

        st_tick = {}

        with ExitStack() as actx:
            def sba(nm, shape, dt):
                return actx.enter_context(nc.sbuf_tensor(name + nm, shape, dt))
            kT = sba("kT", [64, 4, T], BF16)
            Vpad = sba("Vpad", [128, 4, 9, 192], BF16)
            qT = sba("qT", [64, 2, 4, TO], BF16)
            sqb = sba("sqb", [64, TO], F32)
            qraw = sba("qraw", [64, TO], F32)
            E = sba("E", [128, 2, 2, 512], BF16)
            rd = sba("rd", [128, 1, 2, 128], F32)
            t_vz = P.op("dve", lambda: nc.vector.memset(Vpad[:], 0.0), waits=W0, inc=dve_d)
            st = {"sqb_free": None}

            def head_proj_norm(slw, t_w, h4, tok0, ntok, gcol, dst_ap, extra_w=()):
                nts = ntiles_of(ntok)
                s, fr = pss.next()
                t_pe = None
                for k in range(KT):
                    for bi, (n0, nn) in enumerate(nts):
                        lastmm = (k == KT - 1 and bi == len(nts) - 1)
                        t_pe = P.op("pe", lambda k=k, bi=bi, n0=n0, nn=nn, s=s: nc.tensor.matmul(
                            pss.ps[0:64, 4 * s + bi, 0:nn], lhsT=w13[:, slw, k, h4 * 64:(h4 + 1) * 64],
                            rhs=xnT[:, k, tok0 + n0:tok0 + n0 + nn], start=(k == 0), stop=(k == KT - 1)),
                            waits=[t_w, fr, t_norm], inc=pe_u if lastmm else None)
                qps = pss.ps[0:64, 4 * s:4 * s + 4, :].rearrange("p b n -> p (b n)")[:, 0:ntok]
                t_cp = P.op("dve", lambda: nc.vector.tensor_copy(out=qraw[:, 0:ntok], in_=qps), waits=[t_pe, st["sqb_free"]], inc=dve_d)
                pss.release(s, t_cp)
                t_sq = P.op("act", lambda: nc.scalar.activation(out=sqb[:, 0:ntok], in_=qraw[:, 0:ntok], func=AF.Square),
                            waits=[t_cp], inc=act_d)
                s2, fr2 = pss.next()
                t_pe2 = None
                for bi, (n0, nn) in enumerate(nts):
                    t_pe2 = P.op("pe", lambda bi=bi, n0=n0, nn=nn: nc.tensor.matmul(
                        pss.ps[0:64, 4 * s2 + bi, 0:nn], lhsT=ones64[:, :], rhs=sqb[:, n0:n0 + nn], start=True, stop=True),
                        waits=[t_sq, fr2, t_ms], inc=pe_u if bi == len(nts) - 1 else None)
                ssps = pss.ps[0:64, 4 * s2:4 * s2 + 4, :].rearrange("p b n -> p (b n)")[:, 0:ntok]
                t_rt = P.op("act", lambda: nc.scalar.activation(out=sqb[:, 0:ntok], in_=ssps, func=AF.Sqrt,
                                                                scale=1.0 / 64, bias=epsc[0:64, 0:1]),
                            waits=[t_pe2, t_ms], inc=act_d)
                pss.release(s2, t_rt)
                t_rc = P.op("dve", lambda: nc.vector.reciprocal(out=sqb[:, 0:ntok], in_=sqb[:, 0:ntok]), waits=[t_rt], inc=dve_d)
                t_q = P.op("dve", lambda: nc.vector.scalar_tensor_tensor(
                    out=dst_ap, in0=qraw[:, 0:ntok], scalar=gcol[:, 0:1], in1=sqb[:, 0:ntok], op0=ALU.mult, op1=ALU.mult),
                    waits=[t_rc, t_c] + list(extra_w), inc=dve_d)
                st["sqb_free"] = t_q
                return t_q

            slk, t_wk = slab_load(8)
            t_k = None
            for hk in range(4):
                t_k = head_proj_norm(slk, t_wk, hk, 0, TO, kg, kT[:, hk, 128:T])
            slv, t_wv = slab_load(9)
            t_v = None
            blks = list(range(8))
            for u0 in range(0, 8, 4):
                ub = blks[u0:u0 + 4]
                s, fr = pss.next()
                t_pe = None
                for bi, blk in enumerate(ub):
                    for k in range(KT):
                        lastmm = (bi == len(ub) - 1 and k == KT - 1)
                        t_pe = P.op("pe", lambda k=k, bi=bi, blk=blk, s=s: nc.tensor.matmul(
                            pss.ps[:, 4 * s + bi, 0:256], lhsT=xnT[:, k, blk * 128:(blk + 1) * 128], rhs=w13[:, slv, k, :],
                            start=(k == 0), stop=(k == KT - 1)),
                            waits=[t_wv, fr, t_norm], inc=pe_u if lastmm else None)
                for bi, blk in enumerate(ub):
                    t_v = P.op("dve", lambda bi=bi, blk=blk, s=s: nc.vector.tensor_copy(
                        out=Vpad[:, :, blk + 1, 64:128], in_=pss.ps[:, 4 * s + bi, 0:256].rearrange("p (h d) -> p h d", h=4)),
                        waits=[t_pe, t_vz], inc=dve_d)
                pss.release(s, t_v)
                t_pe_v = t_pe
            C.ws13.release(slk, t_pe_v)
            C.ws13.release(slv, t_pe_v)
            q0 = slab_load(0)
            hsem = P.sem(name + "hst")
            hsem2 = P.sem(name + "hld")
            ccs = P.sem(name + "hcc")
            h_out, h_all = halo
            t_hs = P.dma("sp", h_out[0:64, :].rearrange("p (h t) -> p h t", h=4), kT[:, :, TO:T], waits=[t_k], inc=hsem)
            t_hs = P.dma("sp", h_out[64:192, 0:256].rearrange("p (h d) -> p h d", h=4), Vpad[:, :, 8, 64:128], waits=[t_v], inc=hsem)
            t_cc = P.op("pool", lambda: nc.gpsimd.collective_compute("AllGather", ALU.bypass, replica_groups=groups,
                                                                     ins=[h_out[:, :]], outs=[h_all[:, :]]), waits=[t_hs], inc=ccs)
            t_halo = P.dma("sp", kT[:, :, 0:128], h_all[0:64, :].rearrange("p (h t) -> p h t", h=4), waits=[t_cc], inc=hsem2)
            t_halo = P.dma("sp", Vpad[:, :, 0, 64:128], h_all[64:192, 0:256].rearrange("p (h d) -> p h d", h=4), waits=[t_cc, t_vz], inc=hsem2)

            qT_free = [None, None]
            E_free = [None, None]
            rd_free = [None, None]
            cnt = {"e": 0, "r": 0}
            t_last = None
            for j in range(8):
                hk = j // 2
                qb = j % 2
                slq, t_wq = q0 if j == 0 else slab_load(j)
                t_qs = None
                for h4 in range(4):
                    t_qs = head_proj_norm(slq, t_wq, h4, 0, TO, qg, qT[:, qb, h4, :], extra_w=[qT_free[qb]])
                rel_done = {"d": False}

                def scores(n, j=j, hk=hk, qb=qb, t_qs=t_qs, slq=slq, rel_done=rel_done):
                    s, fr = pss.next()
                    eb = cnt["e"] % 2
                    cnt["e"] += 1
                    t_pe = None
                    for kb in range(2):
                        blk = n + kb
                        t_pe = P.op("pe", lambda kb=kb, blk=blk, s=s: nc.tensor.matmul(
                            pss.ps[:, 4 * s + kb, :], lhsT=kT[:, hk, blk * 128:(blk + 1) * 128],
                            rhs=qT[:, qb, :, n * 128:(n + 1) * 128], start=True, stop=True),
                            waits=[fr, t_qs, t_k, t_halo], inc=pe_u if kb == 1 else None)
                    if not rel_done["d"]:
                        C.ws13.release(slq, t_pe)
                        rel_done["d"] = True
                    t_ex = None
                    for kb in range(2):
                        t_ex = P.op("act", lambda kb=kb, s=s, eb=eb: nc.scalar.activation(
                            out=E[:, eb, kb, :], in_=pss.ps[:, 4 * s + kb, :], func=AF.Exp, scale=0.125),
                            waits=[t_pe, E_free[eb]], inc=act_d)
                    pss.release(s, t_ex)
                    t_mk = None
                    for kb in range(2):
                        mk = mk_cur if kb == 1 else (mk_prev0 if n == 0 else mk_prev)
                        t_mk = P.op("dve", lambda kb=kb, eb=eb, mk=mk: nc.vector.tensor_tensor(
                            out=E[:, eb, kb, :].rearrange("p (h q) -> p h q", h=4),
                            in0=E[:, eb, kb, :].rearrange("p (h q) -> p h q", h=4),
                            in1=mk[:, :].unsqueeze(1).to_broadcast([128, 4, 128]), op=ALU.mult),
                            waits=[t_ex, t_c], inc=dve_d)
                    return (n, eb, t_mk, t_pe)

                def pv(sc, j=j, hk=hk):
                    n, eb, t_mk, t_sc = sc
                    s2, fr2 = pss.next()
                    rb = 0
                    t_pv = None
                    for pr in range(2):
                        for bank in range(2):
                            c = 0
                            for hh in range(2):
                                for kb in range(2):
                                    lo = 64 - 64 * hh
                                    lhs = Vpad[:, hk, n + kb, lo:lo + 128] if bank == 0 else onespad[:, lo:lo + 128]
                                    lastmm = (pr == 1 and bank == 1 and c == 3)
                                    t_pv = P.op("pe", lambda lhs=lhs, kb=kb, pr=pr, hh=hh, bank=bank, c=c, s2=s2, eb=eb: nc.tensor.matmul(
                                        pss.ps[:, 4 * s2 + bank, pr * 128:(pr + 1) * 128], lhsT=lhs,
                                        rhs=E[:, eb, kb, (pr * 2 + hh) * 128:(pr * 2 + hh + 1) * 128],
                                        start=(c == 0), stop=(c == 3)),
                                        waits=[t_mk, fr2, t_v], inc=pe_u if lastmm else None)
                                    c += 1
                    E_free[eb] = t_pv
                    t_a = None
                    for pr in range(2):
                        t_a = P.op("dve", lambda pr=pr, rb=rb, s2=s2: nc.vector.tensor_scalar(
                            out=rd[:, rb, pr, :], in0=pss.ps[:, 4 * s2 + 1, pr * 128:(pr + 1) * 128],
                            scalar1=esink[:, 2 * j + pr:2 * j + pr + 1], scalar2=None, op0=ALU.add),
                            waits=[t_pv, rd_free[rb], t_es], inc=dve_d)
                    t_r = P.op("dve", lambda rb=rb: nc.vector.reciprocal(out=rd[:, rb], in_=rd[:, rb]), waits=[t_a], inc=dve_d)
                    t_o = P.op("dve", lambda rb=rb, s2=s2, n=n: nc.vector.tensor_tensor(
                        out=outT[:, 2 * j:2 * j + 2, n * 128:(n + 1) * 128],
                        in0=pss.ps[:, 4 * s2, 0:256].rearrange("p (a q) -> p a q", a=2), in1=rd[:, rb], op=ALU.mult),
                        waits=[t_r], inc=dve_d)
                    pss.release(s2, t_o)
                    rd_free[rb] = t_o
                    return t_o, t_sc

                prev = None
                for n in range(8):
                    cur = scores(n)
                    if prev is not None:
                        t_last, _ = pv(prev)
                    prev = cur
                t_last, t_sc_last = pv(prev)
                qT_free[qb] = t_sc_last
        with ExitStack() as octx:
            def load_wo(n, part=0):
                src = Wout[part][n]
                sl, t = C.ws13.load(lambda t_, sl_: t_[:, sl_].rearrange("p k f -> p (k f)").rearrange("p (k f) -> p k f", k=16), src)
                wv = w13[:, sl].rearrange("p k f -> p (k f)").rearrange("p (k f) -> p k f", k=16)
                return (lambda c: wv[:, c, :]), t, (lambda tk, sl=sl: C.ws13.release(sl, tk))
            if dbg is not None:
                dsem = P.sem(name + "dbga")
                t_last = P.dma("sp", dbg["outA"], outT[:], waits=[t_last], inc=dsem)
            fin_a = accum_rows(P, C, name + "oa", outT, 16, load_wo, x, 0, 1.0, [t_last], st_tick, dve_d, pe_u, octx)

        with ExitStack() as gctx:
            def sbg(nm, shape, dt):
                return gctx.enter_context(nc.sbuf_tensor(name + nm, shape, dt))
            G0 = [fin_a[-1]] + fin_a
            wraw = sbg("wraw", [128, 16, 128], F32)
            wsT = sbg("wsT", [128, 16, 128], BF16)
            bias = sbg("bias", [128, 16, 128], F32)
            lng = sbg("lng", [128, 2, 256], F32)
            lnb = sbg("lnb", [128, 2, 256], F32)
            uT = sbg("uT", [128, 2, TO], F32)
            vf = sbg("vf", [128, 2, 256], F32)
            vsq = sbg("vsq", [128, 256], F32)
            stt = sbg("stt", [128, 2, 8], F32)
            vn = sbg("vn", [128, 8, 256], BF16)
            tmpf = sbg("tmpf", [128, 2, 512], F32)
            gsem = P.sem(name + "gsem")
            lnsem = [P.sem(name + "lns0"), P.sem(name + "lns1")]
            t_g1 = P.dma("sp", wraw[:], aps["ab_w_s"].rearrange("g t s -> t g s"), waits=G0, inc=gsem)
            t_g1 = P.dma("sp", bias[:].rearrange("p g t -> p (g t)"), aps["ab_b_s"].rearrange("g t -> (g t)").partition_broadcast(128),
                         waits=G0, inc=gsem)
            t_ws = None
            for u0 in range(0, 16, 16):
                for q4 in range(4):
                    s, fr = pss.next()
                    t_pe = None
                    for b in range(4):
                        g = q4 * 4 + b
                        t_pe = P.op("pe", lambda g=g, b=b, s=s: nc.tensor.transpose(
                            out=pss.ps[:, 4 * s, b * 128:(b + 1) * 128], in_=wraw[:, g, :], identity=C.ident[:, :]),
                            waits=[t_g1, fr], inc=pe_u if b == 3 else None)
                    t_ws = P.op("dve", lambda q4=q4, s=s: nc.vector.tensor_tensor(
                        out=wsT[:, q4 * 4:q4 * 4 + 4, :], in0=pss.ps[:, 4 * s, :].rearrange("p (g t) -> p g t", g=4),
                        in1=mk_cur[:, :].unsqueeze(1).to_broadcast([128, 4, 128]), op=ALU.mult),
                        waits=[t_pe, t_c], inc=dve_d)
                    pss.release(s, t_ws)
            ln_free = [None, None]
            uT_free = None
            vn_free = None
            vf_free = [None, None]
            tmpf_free = [None, None]
            cntg = {"v": 0, "t": 0}
            t_lastg = None
            for j in range(8):
                lb = j % 2
                t_ln = P.dma("sp", lng[:, lb, :], aps["ab_v_ln_g"][j * 256:(j + 1) * 256].partition_broadcast(128),
                             waits=[ln_free[lb]] + G0, inc=lnsem[lb])
                t_ln = P.dma("sp", lnb[:, lb, :], aps["ab_v_ln_b"][j * 256:(j + 1) * 256].partition_broadcast(128),
                             waits=[ln_free[lb]] + G0, inc=lnsem[lb])
                slu, t_wu = slab_load(10 + j)
                t_u = None
                for gg in range(2):
                    s, fr = pss.next()
                    t_pe = None
                    for k in range(KT):
                        for bi, (n0, nn) in enumerate(nts_o):
                            lastmm = (k == KT - 1 and bi == 1)
                            t_pe = P.op("pe", lambda k=k, bi=bi, n0=n0, nn=nn, s=s, gg=gg, slu=slu: nc.tensor.matmul(
                                pss.ps[:, 4 * s + bi, 0:nn], lhsT=w13[:, slu, k, gg * 128:(gg + 1) * 128],
                                rhs=xnT[:, k, n0:n0 + nn], start=(k == 0), stop=(k == KT - 1)),
                                waits=[t_wu, fr], inc=pe_u if lastmm else None)
                    if gg == 1:
                        C.ws13.release(slu, t_pe)
                    t_u = P.op("act", lambda s=s, gg=gg: nc.scalar.activation(
                        out=uT[:, gg, :], in_=pss.ps[:, 4 * s:4 * s + 2, :].rearrange("p b n -> p (b n)"), func=AF.Gelu),
                        waits=[t_pe, uT_free] + G0, inc=act_d)
                    pss.release(s, t_u)
                slv2, t_wv2 = slab_load(18 + j)
                t_vn = None
                for half in range(2):
                    s, fr = pss.next()
                    t_pe = None
                    for bi in range(4):
                        tt = half * 4 + bi
                        for k in range(KT):
                            lastmm = (bi == 3 and k == KT - 1)
                            t_pe = P.op("pe", lambda k=k, bi=bi, tt=tt, s=s, slv2=slv2: nc.tensor.matmul(
                                pss.ps[:, 4 * s + bi, 0:256], lhsT=xnT[:, k, tt * 128:(tt + 1) * 128],
                                rhs=w13[:, slv2, k, :], start=(k == 0), stop=(k == KT - 1)),
                                waits=[t_wv2, fr], inc=pe_u if lastmm else None)
                    if half == 1:
                        C.ws13.release(slv2, t_pe)
                    t_x = None
                    for bi in range(4):
                        tt = half * 4 + bi
                        vb_ = cntg["v"] % 2
                        cntg["v"] += 1
                        v3 = vf[:, vb_, :].rearrange("p (g c) -> p g c", g=2)
                        t_ge = P.op("act", lambda bi=bi, s=s, vb_=vb_: nc.scalar.activation(
                            out=vf[:, vb_, :], in_=pss.ps[:, 4 * s + bi, 0:256], func=AF.Gelu),
                            waits=[t_pe, vf_free[vb_]] + G0, inc=act_d)
                        t_x = t_ge
                        t1 = P.op("dve", lambda v3=v3, vb_=vb_: nc.vector.tensor_reduce(
                            out=stt[:, vb_, 0:2], in_=v3, axis=AX.X, op=ALU.add), waits=[t_ge], inc=dve_d)
                        t2 = P.op("dve", lambda vb_=vb_: nc.vector.tensor_tensor(
                            out=vsq[:, :], in0=vf[:, vb_, :], in1=vf[:, vb_, :], op=ALU.mult), waits=[t1], inc=dve_d)
                        t3 = P.op("dve", lambda vb_=vb_: nc.vector.tensor_reduce(
                            out=stt[:, vb_, 2:4], in_=vsq[:, :].rearrange("p (g c) -> p g c", g=2), axis=AX.X, op=ALU.add),
                            waits=[t2], inc=dve_d)
                        t4 = P.op("dve", lambda vb_=vb_: nc.vector.tensor_scalar(
                            out=stt[:, vb_, 4:6], in0=stt[:, vb_, 0:2], scalar1=1.0 / 128, scalar2=None, op0=ALU.mult),
                            waits=[t3], inc=dve_d)
                        t5 = P.op("dve", lambda vb_=vb_: nc.vector.tensor_tensor(
                            out=stt[:, vb_, 0:2], in0=stt[:, vb_, 4:6], in1=stt[:, vb_, 4:6], op=ALU.mult), waits=[t4], inc=dve_d)
                        t6 = P.op("dve", lambda vb_=vb_: nc.vector.scalar_tensor_tensor(
                            out=stt[:, vb_, 2:4], in0=stt[:, vb_, 2:4], scalar=1.0 / 128, in1=stt[:, vb_, 0:2],
                            op0=ALU.mult, op1=ALU.subtract), waits=[t5], inc=dve_d)
                        t7 = P.op("act", lambda vb_=vb_: nc.scalar.activation(
                            out=stt[:, vb_, 6:8], in_=stt[:, vb_, 2:4], func=AF.Sqrt, bias=epsc[:, 0:1], scale=1.0),
                            waits=[t6, t_ms], inc=act_d)
                        t8 = P.op("dve", lambda vb_=vb_: nc.vector.reciprocal(out=stt[:, vb_, 6:8], in_=stt[:, vb_, 6:8]),
                                  waits=[t7], inc=dve_d)
                        t9 = None
                        for gg in range(2):
                            t9 = P.op("dve", lambda vb_=vb_, gg=gg: nc.vector.tensor_scalar(
                                out=vf[:, vb_, gg * 128:(gg + 1) * 128], in0=vf[:, vb_, gg * 128:(gg + 1) * 128],
                                scalar1=stt[:, vb_, 4 + gg:5 + gg], scalar2=stt[:, vb_, 6 + gg:7 + gg],
                                op0=ALU.subtract, op1=ALU.mult), waits=[t8], inc=dve_d)
                        t10 = P.op("dve", lambda vb_=vb_, lb=lb: nc.vector.tensor_tensor(
                            out=vf[:, vb_, :], in0=vf[:, vb_, :], in1=lng[:, lb, :], op=ALU.mult), waits=[t9, t_ln], inc=dve_d)
                        t_vn = P.op("dve", lambda vb_=vb_, lb=lb, tt=tt: nc.vector.tensor_tensor(
                            out=vn[:, tt, :], in0=vf[:, vb_, :], in1=lnb[:, lb, :], op=ALU.add),
                            waits=[t10, vn_free], inc=dve_d)
                        vf_free[vb_] = t_vn
                    pss.release(s, t_x)
                ln_free[lb] = t_vn
                t_sp = None
                for gg in range(2):
                    g = 2 * j + gg
                    for half in range(2):
                        s, fr = pss.next()
                        t_pe = None
                        for c4 in range(4):
                            tt = half * 4 + c4
                            t_pe = P.op("pe", lambda tt=tt, c4=c4, s=s, gg=gg, g=g: nc.tensor.matmul(
                                pss.ps[:, 4 * s, c4 * 128:(c4 + 1) * 128], lhsT=vn[:, tt, gg * 128:(gg + 1) * 128],
                                rhs=wsT[:, g, :], start=True, stop=True),
                                waits=[t_vn, t_ws, fr], inc=pe_u if c4 == 3 else None)
                        t_sp = t_pe
                        tb = cntg["t"] % 2
                        cntg["t"] += 1
                        t_b = P.op("dve", lambda s=s, g=g, tb=tb: nc.vector.tensor_tensor(
                            out=tmpf[:, tb, :].rearrange("p (a t) -> p a t", a=4),
                            in0=pss.ps[:, 4 * s, :].rearrange("p (a t) -> p a t", a=4),
                            in1=bias[:, g, :].unsqueeze(1).to_broadcast([128, 4, 128]), op=ALU.add),
                            waits=[t_pe, t_g1, tmpf_free[tb]], inc=dve_d)
                        pss.release(s, t_b)
                        t_lastg = P.op("dve", lambda g=g, gg=gg, half=half, tb=tb: nc.vector.tensor_tensor(
                            out=outT[:, g, half * 512:(half + 1) * 512], in0=tmpf[:, tb, :],
                            in1=uT[:, gg, half * 512:(half + 1) * 512], op=ALU.mult),
                            waits=[t_b, t_u], inc=dve_d)
                        tmpf_free[tb] = t_lastg
                uT_free = t_lastg
                vn_free = t_sp
        with ExitStack() as octx:
            if dbg is not None:
                dsem2 = P.sem(name + "dbgb")
                t_lastg = P.dma("sp", dbg["outB"], outT[:], waits=[t_lastg], inc=dsem2)
            fin_b = accum_rows(P, C, name + "ob", outT, 16, lambda n: load_wo(n, 1), x, 0, 1.0, [t_lastg], st_tick, dve_d, pe_u, octx)
    return fin_b


CL = 32
NCHK = 1024 // CL


def hgrn_consts(P, C, ctx, aps, waits, name):
    nc = P.nc
    pss = C.pss
    pe_d = P.shared("norm_pe")
    dve_d = P.shared("norm_dve")
    act_d = P.shared("norm_act")
    sem = P.shared("norm_g")
    H = Ctx()
    p0, t0 = load_colvec(P, ctx, aps["c_lb_logits"][0], KT, C.ident, pe_d, dve_d, pss, name + "p0", sem, waits=waits)
    sem2 = P.sem(name + "ld2")
    p1, t1 = load_colvec(P, ctx, aps["c_lb_logits"][1], KT, C.ident, pe_d, dve_d, pss, name + "p1", sem2, waits=waits)
    H.lbc = ctx.enter_context(nc.sbuf_tensor(name + "lbc", [128, KT], F32))
    H.oml = ctx.enter_context(nc.sbuf_tensor(name + "oml", [128, KT], F32))
    ta = P.op("dve", lambda: nc.vector.tensor_tensor(out=H.lbc[:], in0=p1[:], in1=p0[:], op=ALU.subtract), waits=[t0, t1], inc=dve_d)
    tb = P.op("act", lambda: nc.scalar.activation(out=H.lbc[:], in_=H.lbc[:], func=AF.Sigmoid), waits=[ta], inc=act_d)
    H.t = P.op("dve", lambda: nc.vector.tensor_scalar(out=H.oml[:], in0=H.lbc[:], scalar1=-1.0, scalar2=1.0, op0=ALU.mult, op1=ALU.add),
               waits=[tb], inc=dve_d)
    return H


def hgrn_pass1(P, C, x, aps, scr, x_ready, name):
    from contextlib import ExitStack
    nc = P.nc
    pss = C.pss
    xnT = C.xnT
    w13 = C.w13
    T = 1024
    nts = ntiles_of(T)
    Winr = aps["c_w_in"]
    psb = C.ps[:].rearrange("p b n -> p (b n)").bitcast(BF16).rearrange("p (b n) -> p b n", b=8)
    with ExitStack() as ctx:
        pe_u = P.sem(name + "peu")
        act_d = P.sem(name + "actd")
        dve_d = P.sem(name + "dved")
        csem = P.sem(name + "csem")
        t_norm = rmsnorm_T(P, ctx, pss, x, T, aps["mix_norm"], C.ident, xnT, x_ready, name + "n")
        W0 = [t_norm]
        H = hgrn_consts(P, C, ctx, aps, W0, name + "hc")

        def sb(nm, shape, dt):
            return ctx.enter_context(nc.sbuf_tensor(name + nm, shape, dt))
        segm = sb("segm", [128, T], BF16)
        onec = sb("onec", [128, 1], F32)
        zeroc = sb("zeroc", [128, 1], F32)
        bdm = sb("bdm", [128, 128], F32)
        rowm = sb("rowm", [128, 4], F32)
        qf = sb("qf", [128, T], F32)
        bA = sb("bA", [128, T], F32)
        kk = sb("kk", [128, T], F32)
        cg = sb("cg", [128, T], F32)
        bD = sb("bD", [128, T], F32)
        ex = sb("ex", [128, 2, T], F32)
        qtil = sb("qtil", [128, 2, T], BF16)
        qbar = sb("qbar", [128, 2, T], BF16)
        qhat = sb("qhat", [128, 2, T], BF16)
        ktil = sb("ktil", [128, 2, T], BF16)
        khatT = sb("khatT", [128, T], BF16)
        khm = sb("khm", [128, 2, 8, 4, 128], BF16)
        vtok = sb("vtok", [128, 8, 2, 128], BF16)
        attm = sb("attm", [128, 2, 128], BF16)
        Sm = sb("Sm", [128, 2, 2, 128], F32)
        Sbf = sb("Sbf", [128, 2, 8, 128], BF16)
        Dc = sb("Dc", [128, 2, NCHK], F32)
        ost = sb("ost", [128, 2, T], F32)

        t_c = P.dma("sp", bdm[:], aps["bdmask"], waits=W0, inc=csem)
        t_c = P.dma("sp", rowm[:], aps["rowmask"], waits=W0, inc=csem)
        P.op("dve", lambda: nc.vector.memset(segm[:], 1.0), waits=W0)
        P.op("dve", lambda: nc.vector.memset(segm[:].rearrange("p (c l) -> p c l", l=CL)[:, :, 0:1], 0.0))
        P.op("dve", lambda: nc.vector.memset(onec[:], 1.0))
        t_ms = P.op("dve", lambda: nc.vector.memset(zeroc[:], 0.0), inc=dve_d)

        def slab_load(j):
            return C.ws13.load(lambda t, sl: t[:, sl], Winr[j])

        def proj_fm(slw, t_w, hh, evac):
            s, fr = pss.next()
            t_pe = None
            for k in range(KT):
                for bi, (n0, nn) in enumerate(nts):
                    lastmm = (k == KT - 1 and bi == 1)
                    t_pe = P.op("pe", lambda k=k, bi=bi, n0=n0, nn=nn, s=s: nc.tensor.matmul(
                        pss.ps[:, 4 * s + bi, 0:nn], lhsT=w13[:, slw, k, hh * 128:(hh + 1) * 128],
                        rhs=xnT[:, k, n0:n0 + nn], start=(k == 0), stop=(k == KT - 1)),
                        waits=[t_w, fr, t_norm], inc=pe_u if lastmm else None)
            psv = pss.ps[:, 4 * s:4 * s + 2, :].rearrange("p b n -> p (b n)")
            t_rel = evac(psv, t_pe)
            pss.release(s, t_rel)
            return t_pe

        st = {"qf": None, "bA": None, "kk": None, "cg": None, "bD": None, "ex": [None, None], "exi": 0,
              "khatT": None, "vtok": None, "ost": [None, None], "osem": [P.sem(name + "os0"), P.sem(name + "os1")],
              "qh": [None, None], "qsem": [P.sem(name + "qs0"), P.sem(name + "qs1")], "ssem": [P.sem(name + "ss0"), P.sem(name + "ss1")],
              "Sm": [None, None], "tilfree": [None, None], "khm": [None, None], "attm": [None, None]}
        finals = []
        cg3 = cg[:].rearrange("p (c l) -> p c l", l=CL)

        def next_ex():
            i = st["exi"] % 2
            st["exi"] += 1
            return i

        for sp in range(16):
            slq, t_wq = slab_load(sp)
            slz, t_wz = slab_load(16 + sp)
            sli, t_wi = slab_load(32 + sp)
            t_vt = None
            for half in range(2):
                s, fr = pss.next()
                t_pe = None
                for bi in range(4):
                    tt = half * 4 + bi
                    for k in range(KT):
                        lastmm = (bi == 3 and k == KT - 1)
                        t_pe = P.op("pe", lambda k=k, bi=bi, tt=tt, s=s, sli=sli: nc.tensor.matmul(
                            pss.ps[:, 4 * s + bi, 0:256], lhsT=xnT[:, k, tt * 128:(tt + 1) * 128], rhs=w13[:, sli, k, :],
                            start=(k == 0), stop=(k == KT - 1)),
                            waits=[t_wi, fr, t_norm], inc=pe_u if lastmm else None)
                if half == 1:
                    C.ws13.release(sli, t_pe)
                t_vt = P.op("act", lambda s=s, half=half: nc.scalar.activation(
                    out=vtok[:, half * 4:half * 4 + 4, :, :].rearrange("p t h e -> p t (h e)"),
                    in_=pss.ps[:, 4 * s:4 * s + 4, 0:256], func=AF.Copy),
                    waits=[t_pe, st["vtok"]], inc=act_d)
                pss.release(s, t_vt)
            per_head = []
            for hh in range(2):
                h = 2 * sp + hh
                def ev_q(psv, t_pe):
                    t = P.op("act", lambda: nc.scalar.activation(out=qf[:], in_=psv, func=AF.Copy), waits=[t_pe, st["qf"]], inc=act_d)
                    return t
                t_pq = proj_fm(slq, t_wq, hh, ev_q)
                t_qf = (act_d, act_d.n)
                def ev_z(psv, t_pe):
                    return P.op("act", lambda: nc.scalar.activation(out=bA[:], in_=psv, func=AF.Sigmoid), waits=[t_pe, st["bA"]], inc=act_d)
                t_pz = proj_fm(slz, t_wz, hh, ev_z)
                if hh == 1:
                    C.ws13.release(slq, t_pz)
                    C.ws13.release(slz, t_pz)
                t_sg = (act_d, act_d.n)
                t_f = P.op("dve", lambda h=h: nc.vector.tensor_scalar(out=bA[:], in0=bA[:], scalar1=H.oml[:, h:h + 1], scalar2=H.lbc[:, h:h + 1],
                                                                    op0=ALU.mult, op1=ALU.add), waits=[t_sg, H.t], inc=dve_d)
                t_kk = P.op("dve", lambda: nc.vector.tensor_scalar(out=kk[:], in0=bA[:], scalar1=-1.0, scalar2=1.0, op0=ALU.mult, op1=ALU.add),
                            waits=[t_f, st["kk"]], inc=dve_d)
                t_eg = P.op("dve", lambda: nc.vector.tensor_tensor_scan(out=bD[:], data0=bA[:], data1=zeroc[:, 0:1].to_broadcast([128, T]),
                                                                       initial=1.0, op0=ALU.mult, op1=ALU.add),
                            waits=[t_kk, t_ms, st["bD"]], inc=dve_d)
                t_qh = P.op("dve", lambda hh=hh: nc.vector.tensor_tensor(out=qhat[:, hh, :], in0=qf[:], in1=bD[:], op=ALU.mult),
                            waits=[t_eg, t_qf, st["qh"][hh]], inc=dve_d)
                st["bD"] = t_qh
                t_qst = P.dma("sp", scr["qhat"][h], qhat[:, hh, :], waits=[t_qh], inc=st["qsem"][hh])
                st["qh"][hh] = t_qst
                finals.append(t_qst)
                t_ln = P.op("act", lambda: nc.scalar.activation(out=bA[:], in_=bA[:], func=AF.Ln), waits=[t_eg], inc=act_d)
                t_cg = P.op("dve", lambda: nc.vector.tensor_tensor_scan(out=cg[:], data0=segm[:], data1=bA[:], initial=0.0,
                                                                       op0=ALU.mult, op1=ALU.add), waits=[t_ln, st["cg"]], inc=dve_d)
                st["bA"] = t_cg
                t_dc = P.op("act", lambda hh=hh: nc.scalar.activation(out=Dc[:, hh, :], in_=cg3[:, :, CL - 1], func=AF.Exp),
                            waits=[t_cg, st["Sm"][hh]], inc=act_d)
                e0 = next_ex()
                t_e = P.op("act", lambda e0=e0: nc.scalar.activation(out=ex[:, e0, :], in_=cg[:], func=AF.Exp), waits=[t_cg, st["ex"][e0]], inc=act_d)
                t_qb = P.op("dve", lambda e0=e0, hh=hh: nc.vector.tensor_tensor(out=qbar[:, hh, :], in0=qf[:], in1=ex[:, e0, :], op=ALU.mult),
                            waits=[t_e, st["tilfree"][hh]], inc=dve_d)
                st["ex"][e0] = t_qb
                t_d1 = P.op("dve", lambda: nc.vector.tensor_tensor(out=bA[:].rearrange("p (c l) -> p c l", l=CL), in0=cg3,
                                                                  in1=cg3[:, :, CL // 2 - 1:CL // 2].to_broadcast([128, NCHK, CL]), op=ALU.subtract),
                            waits=[t_cg], inc=dve_d)
                e1 = next_ex()
                t_e = P.op("act", lambda e1=e1: nc.scalar.activation(out=ex[:, e1, :], in_=bA[:], func=AF.Exp), waits=[t_d1, st["ex"][e1]], inc=act_d)
                t_qt = P.op("dve", lambda e1=e1, hh=hh: nc.vector.tensor_tensor(out=qtil[:, hh, :], in0=qf[:], in1=ex[:, e1, :], op=ALU.mult),
                            waits=[t_e], inc=dve_d)
                st["ex"][e1] = t_qt
                st["qf"] = t_qt
                e2 = next_ex()
                t_e = P.op("act", lambda e2=e2: nc.scalar.activation(out=ex[:, e2, :], in_=bA[:], func=AF.Exp, scale=-1.0),
                           waits=[t_d1, st["ex"][e2]], inc=act_d)
                t_kt = P.op("dve", lambda e2=e2, hh=hh: nc.vector.tensor_tensor(out=ktil[:, hh, :], in0=kk[:], in1=ex[:, e2, :], op=ALU.mult),
                            waits=[t_e], inc=dve_d)
                st["ex"][e2] = t_kt
                t_d2 = P.op("dve", lambda: nc.vector.tensor_tensor(out=bA[:].rearrange("p (c l) -> p c l", l=CL),
                                                                  in0=cg3[:, :, CL - 1:CL].to_broadcast([128, NCHK, CL]), in1=cg3, op=ALU.subtract),
                            waits=[t_e], inc=dve_d)
                st["cg"] = t_d2
                e3 = next_ex()
                t_e = P.op("act", lambda e3=e3: nc.scalar.activation(out=ex[:, e3, :], in_=bA[:], func=AF.Exp), waits=[t_d2, st["ex"][e3]], inc=act_d)
                st["bA"] = t_e
                t_kh = P.op("dve", lambda e3=e3: nc.vector.tensor_tensor(out=khatT[:], in0=kk[:], in1=ex[:, e3, :], op=ALU.mult),
                            waits=[t_e, st["khatT"]], inc=dve_d)
                st["ex"][e3] = t_kh
                st["kk"] = t_kh
                s, fr = pss.next()
                t_pe = None
                for tt in range(8):
                    t_pe = P.op("pe", lambda tt=tt, s=s: nc.tensor.transpose(out=psb[:, 4 * s, tt * 128:(tt + 1) * 128],
                                                                             in_=khatT[:, tt * 128:(tt + 1) * 128], identity=C.identb[:, :]),
                                waits=[t_kh, fr], inc=pe_u if tt == 7 else None)
                st["khatT"] = t_pe
                t_km = None
                for c4 in range(4):
                    t_km = P.op("act", lambda c4=c4, s=s, hh=hh: nc.scalar.activation(
                        out=khm[:, hh, :, c4, :], in_=psb[:, 4 * s, :].rearrange("p (t d) -> p t d", t=8), func=AF.Copy,
                        scale=rowm[:, c4:c4 + 1]), waits=[t_pe, t_c, st["khm"][hh]], inc=act_d)
                pss.release(s, t_km)
                t_s0 = P.op("dve", lambda hh=hh: nc.vector.memset(Sm[:, hh, 0, :], 0.0), waits=[st["Sm"][hh]], inc=dve_d)
                per_head.append(dict(h=h, hh=hh, t_ops=[t_qb, t_qt, t_kt, t_km, t_vt], t_dc=t_dc, t_s0=t_s0))
            lastS = [None, None]
            cp_hist = [[None, None], [None, None]]
            last_rd = [None, None]
            t_oev = [None, None]
            for tt in range(8):
                ph1 = []
                for hd in per_head:
                    hh = hd["hh"]
                    s, fr = pss.next()
                    t_at = P.op("pe", lambda s=s, hh=hh, tt=tt: nc.tensor.matmul(
                        pss.ps[:, 4 * s, 0:128], lhsT=ktil[:, hh, tt * 128:(tt + 1) * 128], rhs=qtil[:, hh, tt * 128:(tt + 1) * 128],
                        start=True, stop=True), waits=hd["t_ops"] + [fr], inc=pe_u)
                    t_kv = None
                    for c4 in range(4):
                        t_kv = P.op("pe", lambda s=s, hh=hh, tt=tt, c4=c4: nc.tensor.matmul(
                            pss.ps[:, 4 * s + 1, c4 * 128:(c4 + 1) * 128], lhsT=khm[:, hh, tt, c4, :], rhs=vtok[:, tt, hh, :],
                            start=True, stop=True), inc=pe_u if c4 == 3 else None)
                    t_am = P.op("dve", lambda s=s, hh=hh: nc.vector.tensor_tensor(out=attm[:, hh, :], in0=pss.ps[:, 4 * s, 0:128], in1=bdm[:],
                                                                             op=ALU.mult), waits=[t_at, t_c, st["attm"][hh]], inc=dve_d)
                    ph1.append((hd, s, t_am, t_kv))
                for hd, s, t_am, t_kv in ph1:
                    hh = hd["hh"]
                    t_oi = P.op("pe", lambda s=s, hh=hh, tt=tt: nc.tensor.matmul(
                        pss.ps[:, 4 * s + 2, 0:128], lhsT=vtok[:, tt, hh, :], rhs=attm[:, hh, :], start=True, stop=False),
                        waits=[t_am], inc=pe_u)
                    st["attm"][hh] = t_oi
                    t_in = t_oi
                    for c4 in range(4):
                        c = tt * 4 + c4
                        slot = c % 8
                        if c > 0:
                            t_in = P.op("pe", lambda s=s, hh=hh, tt=tt, c4=c4, slot=slot: nc.tensor.matmul(
                                pss.ps[:, 4 * s + 2, c4 * CL:(c4 + 1) * CL], lhsT=Sbf[:, hh, slot, :],
                                rhs=qbar[:, hh, tt * 128 + c4 * CL:tt * 128 + (c4 + 1) * CL], start=False, stop=(c4 == 3)),
                                waits=[lastS[hh]], inc=pe_u)
                        t_up = P.op("dve", lambda s=s, hh=hh, c=c, c4=c4: nc.vector.scalar_tensor_tensor(
                            out=Sm[:, hh, (c + 1) % 2, :], in0=Sm[:, hh, c % 2, :], scalar=Dc[:, hh, c:c + 1],
                            in1=pss.ps[:, 4 * s + 1, c4 * 128:(c4 + 1) * 128],
                            op0=ALU.mult, op1=ALU.add), waits=[t_kv, hd["t_dc"], hd["t_s0"], cp_hist[hh][0]], inc=dve_d)
                        nslot = (c + 1) % 8
                        lastS[hh] = P.op("act", lambda hh=hh, nslot=nslot, c=c: nc.scalar.activation(out=Sbf[:, hh, nslot, :], in_=Sm[:, hh, (c + 1) % 2, :],
                                                                                                func=AF.Copy),
                                         waits=[t_up, last_rd[hh]], inc=act_d)
                        cp_hist[hh] = [cp_hist[hh][1], lastS[hh]]
                        t_lastup = t_up
                    last_rd[hh] = t_in
                    t_oev[hh] = P.op("act", lambda s=s, hh=hh, tt=tt: nc.scalar.activation(out=ost[:, hh, tt * 128:(tt + 1) * 128],
                                                                                      in_=pss.ps[:, 4 * s + 2, 0:128], func=AF.Copy),
                                     waits=[t_in, st["ost"][hh]], inc=act_d)
                    pss.release(s, t_oev[hh])
                    hd["t_lastup"] = t_lastup
            for hd in per_head:
                hh = hd["hh"]
                h = hd["h"]
                t_os = P.dma("sp", scr["oloc"][h], ost[:, hh, :], waits=[t_oev[hh]], inc=st["osem"][hh])
                st["ost"][hh] = t_os
                t_ss = P.dma("sp", scr["sfin"][h], Sm[:, hh, 0, :], waits=[hd["t_lastup"]], inc=st["ssem"][hh])
                st["Sm"][hh] = t_ss
                st["tilfree"][hh] = last_rd[hh]
                st["khm"][hh] = last_rd[hh]
                finals += [t_os, t_ss]
            st["vtok"] = last_rd[1]
    return finals


def hgrn_pass2(P, C, x, row0, aps, scr, sprev, ready, name, renorm_src=None):
    from contextlib import ExitStack
    nc = P.nc
    pss = C.pss
    xnT = C.xnT
    w13 = C.w13
    T = 1024
    nts = ntiles_of(T)
    Winr = aps["c_w_in"]
    Wout = aps["c_w_out"]
    with ExitStack() as ctx:
        pe_u = P.sem(name + "peu")
        act_d = P.sem(name + "actd")
        dve_d = P.sem(name + "dved")
        csem = P.sem(name + "csem")
        W0 = list(ready)
        if renorm_src is not None:
            t_norm = rmsnorm_T(P, ctx, pss, renorm_src, T, aps["mix_norm"], C.ident, xnT, ready, name + "n")
            W0 = [t_norm]

        def sb(nm, shape, dt):
            return ctx.enter_context(nc.sbuf_tensor(name + nm, shape, dt))
        ogc = sb("ogc", [128, 1], F32)
        outT = sb("outT", [128, 16, T], BF16)
        flagc = sb("flagc", [128, 1], F32)
        t_c = P.dma("sp", ogc[:], aps["ogain"], waits=W0, inc=csem)
        t_c = P.dma("sp", flagc[:], aps["flag"], waits=W0, inc=csem)
        st_tick = {}
        fin = None
        t_prev_part = W0
        for part in range(2):
            with ExitStack() as pctx:
                def sbp(nm, shape, dt):
                    return pctx.enter_context(nc.sbuf_tensor(name + nm + str(part), shape, dt))
                of = sbp("of", [128, 2, T], F32)
                qh = sbp("qh", [128, 2, T], BF16)
                s32 = sbp("s32", [128, 2, 128], F32)
                sbf = sbp("sbf", [128, 2, 128], BF16)
                sgt = sbp("sgt", [128, T], F32)
                sqf = sbp("sqf", [128, T], F32)
                ldo = [P.shared(name + f"lo{i}") for i in range(2)]
                ldq = [P.shared(name + f"lq{i}") for i in range(2)]
                lds = [P.shared(name + f"ls{i}") for i in range(2)]
                fr_of = [None, None]
                fr_qh = [None, None]
                fr_s32 = [None, None]
                fr_sbf = [None, None]
                fr_sgt = None
                fr_sqf = None
                t_last = None
                for hl in range(16):
                    h = part * 16 + hl
                    b = hl % 2
                    if hl % 2 == 0:
                        slg, t_wg = C.ws13.load(lambda t, sl: t[:, sl], Winr[48 + h // 2])
                    hh = h % 2
                    t_lo = P.dma("sp", of[:, b, :], scr["oloc"][h], waits=[fr_of[b]] + list(t_prev_part), inc=ldo[b])
                    t_lq = P.dma("sp", qh[:, b, :], scr["qhat"][h], waits=[fr_qh[b]] + list(t_prev_part), inc=ldq[b])
                    t_ls = P.dma("sp", s32[:, b, :], sprev[h], waits=[fr_s32[b]] + list(t_prev_part), inc=lds[b])
                    t_sb = P.op("dve", lambda b=b: nc.vector.tensor_scalar(out=sbf[:, b, :], in0=s32[:, b, :], scalar1=flagc[:, 0:1], scalar2=None,
                                                                          op0=ALU.mult), waits=[t_ls, fr_sbf[b], t_c], inc=dve_d)
                    fr_s32[b] = t_sb
                    s, fr = pss.next()
                    t_pe = None
                    for k in range(KT):
                        for bi, (n0, nn) in enumerate(nts):
                            lastmm = (k == KT - 1 and bi == 1)
                            t_pe = P.op("pe", lambda k=k, bi=bi, n0=n0, nn=nn, s=s, slg=slg, hh=hh: nc.tensor.matmul(
                                pss.ps[:, 4 * s + bi, 0:nn], lhsT=w13[:, slg, k, hh * 128:(hh + 1) * 128],
                                rhs=xnT[:, k, n0:n0 + nn], start=(k == 0), stop=(k == KT - 1)),
                                waits=[t_wg, fr] + W0, inc=pe_u if lastmm else None)
                    if hh == 1:
                        C.ws13.release(slg, t_pe)
                    t_sg = P.op("act", lambda s=s: nc.scalar.activation(out=sgt[:], in_=pss.ps[:, 4 * s:4 * s + 2, :].rearrange("p b n -> p (b n)"),
                                                                        func=AF.Silu), waits=[t_pe, fr_sgt] + list(t_prev_part), inc=act_d)
                    pss.release(s, t_sg)
                    s, fr = pss.next()
                    t_pc = None
                    for bi, (n0, nn) in enumerate(nts):
                        t_pc = P.op("pe", lambda bi=bi, n0=n0, nn=nn, s=s, b=b: nc.tensor.matmul(
                            pss.ps[:, 4 * s + bi, 0:nn], lhsT=sbf[:, b, :], rhs=qh[:, b, n0:n0 + nn], start=True, stop=True),
                            waits=[t_sb, t_lq, fr], inc=pe_u if bi == 1 else None)
                    fr_qh[b] = t_pc
                    fr_sbf[b] = t_pc
                    t_o = P.op("dve", lambda s=s, b=b: nc.vector.tensor_tensor(
                        out=of[:, b, :], in0=pss.ps[:, 4 * s:4 * s + 2, :].rearrange("p b n -> p (b n)"), in1=of[:, b, :], op=ALU.add),
                        waits=[t_pc, t_lo], inc=dve_d)
                    pss.release(s, t_o)
                    t_sq = P.op("act", lambda b=b: nc.scalar.activation(out=sqf[:], in_=of[:, b, :], func=AF.Square), waits=[t_o, fr_sqf], inc=act_d)
                    s2, fr2 = pss.next()
                    t_p2 = None
                    for bi, (n0, nn) in enumerate(nts):
                        t_p2 = P.op("pe", lambda bi=bi, n0=n0, nn=nn, s2=s2: nc.tensor.matmul(
                            pss.ps[:, 4 * s2 + bi, 0:nn], lhsT=C.ones128[:, :], rhs=sqf[:, n0:n0 + nn], start=True, stop=True),
                            waits=[t_sq, fr2, C.t_ident], inc=pe_u if bi == 1 else None)
                    t_rt = P.op("act", lambda s2=s2: nc.scalar.activation(out=sqf[:], in_=pss.ps[:, 4 * s2:4 * s2 + 2, :].rearrange("p b n -> p (b n)"),
                                                                          func=AF.Sqrt, scale=1.0 / 128, bias=C.epsc[:, 0:1]), waits=[t_p2], inc=act_d)
                    pss.release(s2, t_rt)
                    t_rc = P.op("dve", lambda: nc.vector.reciprocal(out=sqf[:], in_=sqf[:]), waits=[t_rt], inc=dve_d)
                    t_m1 = P.op("dve", lambda b=b: nc.vector.scalar_tensor_tensor(out=of[:, b, :], in0=of[:, b, :], scalar=ogc[:, 0:1], in1=sqf[:],
                                                                                op0=ALU.mult, op1=ALU.mult), waits=[t_rc, t_c], inc=dve_d)
                    fr_sqf = t_m1
                    t_last = P.op("dve", lambda b=b, hl=hl: nc.vector.tensor_tensor(out=outT[:, hl, :], in0=of[:, b, :], in1=sgt[:], op=ALU.mult),
                                  waits=[t_m1, t_sg], inc=dve_d)
                    fr_of[b] = t_last
                    fr_sgt = t_last
            with ExitStack() as octx:
                def load_wo(n, part=part):
                    src = Wout[part][n]
                    sl, t = C.ws13.load(lambda t_, sl_: t_[:, sl_].rearrange("p k f -> p (k f)").rearrange("p (k f) -> p k f", k=16), src)
                    wv = w13[:, sl].rearrange("p k f -> p (k f)").rearrange("p (k f) -> p k f", k=16)
                    return (lambda c: wv[:, c, :]), t, (lambda tk, sl=sl: C.ws13.release(sl, tk))
                fin = accum_rows(P, C, name + f"o{part}", outT, 16, load_wo, x, row0, 1.0, [t_last], st_tick, dve_d, pe_u, octx)
                t_prev_part = [fin[-1], fin[-2], fin[-3]]
    return fin


N_CORES = 8
CONST_SHAPES = {"ident": [128, 128], "mk_cur": [128, 128], "mk_prev": [128, 128], "mk_prev0": [128, 128], "sinks_l": [128, 16],
                "qg": [64, 1], "kg": [64, 1], "ogain": [128, 1], "bdmask": [128, 128], "rowmask": [128, 4], "flag": [128, 1]}


def weight_shapes(ff):
    npr, nch = ff // 256, ff // 128
    return {"ffn1_norm": [2, D], "ffn1_w1": [2, npr, 128, KT, 256], "ffn1_w3": [2, npr, 128, KT, 256], "ffn1_w2": [2, 8, 128, nch, 512],
            "mix_norm": [2, D],
            "ffn2_norm": [2, D], "ffn2_w1": [2, npr, 128, KT, 256], "ffn2_w3": [2, npr, 128, KT, 256], "ffn2_w2": [2, 8, 128, nch, 512],
            "ab_w_in": [26, 128, KT, 256], "ab_v_ln_g": [1, 2048], "ab_v_ln_b": [1, 2048], "ab_w_s": [1, 16, 128, 128], "ab_b_s": [1, 16, 128],
            "ab_w_out": [2, 8, 128, 16, 512], "c_w_in": [64, 128, KT, 256], "c_lb_logits": [2, D], "c_w_out": [2, 8, 128, 16, 512]}


def tile_in(w):
    lead = w.shape[:-2]
    F = w.shape[-1]
    v = w.reshape(*lead, KT, 128, F // 256, 256)
    nl = len(lead)
    return np.ascontiguousarray(np.transpose(v, (*range(nl), nl + 2, nl + 1, nl, nl + 3)))


def tile_w2(w):
    lead = w.shape[:-2]
    R = w.shape[-2]
    v = w.reshape(*lead, R // 128, 128, 8, 512)
    nl = len(lead)
    return np.ascontiguousarray(np.transpose(v, (*range(nl), nl + 2, nl + 1, nl, nl + 3)))


def tile_out(w):
    v = w.reshape(2, 16, 128, 8, 512)
    return np.ascontiguousarray(np.transpose(v, (0, 3, 2, 1, 4)))


def build_program(ff=FF, n_cores=N_CORES):
    from contextlib import ExitStack
    nc = bass.Bass("TRN2", target_bir_lowering=False)
    xin = nc.dram_tensor("xin", [1024, D], F32, kind="ExternalInput").ap()
    w = {n: nc.dram_tensor(n, s, F32, kind="ExternalInput").ap() for n, s in weight_shapes(ff).items()}
    cst = {n: nc.dram_tensor(n, s, F32, kind="ExternalInput").ap() for n, s in CONST_SHAPES.items()}
    out = nc.dram_tensor("out", [1024, D], F32, kind="ExternalOutput").ap()
    xres = nc.dram_tensor("xres", [1024, D], F32, kind="Internal").ap()
    h_out = nc.dram_tensor("halo_out", [192, 512], BF16, kind="Internal").ap()
    h_all = nc.dram_tensor("halo_all", [384, 512], BF16, kind="Internal", addr_space="Local").ap()
    oloc = nc.dram_tensor("oloc", [32, 128, 1024], F32, kind="Internal").ap()
    qhat = nc.dram_tensor("qhat", [32, 128, 1024], BF16, kind="Internal").ap()
    sfin = nc.dram_tensor("sfin", [4096, 128], F32, kind="Internal").ap()
    sall = nc.dram_tensor("sall", [8192, 128], F32, kind="Internal", addr_space="Local").ap()
    nch = ff // 128
    P = Prog(nc)
    with ExitStack() as st:
        C = make_ctx(P, st, cst["ident"])
        xo = xres
        groups = [[2 * i, 2 * i + 1] for i in range(n_cores // 2)]
        f = ffn_stage(P, C, xin, xres, 1024, w["ffn1_norm"][0], w["ffn1_w1"][0], w["ffn1_w3"][0], w["ffn1_w2"][0], [C.t_ident], "fa0", nch=nch)
        ab_aps = {"mix_norm": w["mix_norm"][0], "ab_w_in": w["ab_w_in"], "ab_w_out": w["ab_w_out"], "ab_v_ln_g": w["ab_v_ln_g"][0],
                  "ab_v_ln_b": w["ab_v_ln_b"][0], "ab_w_s": w["ab_w_s"][0], "ab_b_s": w["ab_b_s"][0]}
        for k in ("qg", "kg", "sinks_l", "mk_cur", "mk_prev", "mk_prev0"):
            ab_aps[k] = cst[k]
        f = ab_stage(P, C, xres, ab_aps, f, "ab", (h_out, h_all), groups)
        f = ffn_stage(P, C, xo, xo, 1024, w["ffn2_norm"][0], w["ffn2_w1"][0], w["ffn2_w3"][0], w["ffn2_w2"][0], f, "fb0", nch=nch)
        f = ffn_stage(P, C, xo, xo, 1024, w["ffn1_norm"][1], w["ffn1_w1"][1], w["ffn1_w3"][1], w["ffn1_w2"][1], f, "fa1", nch=nch)
        h_aps = {"mix_norm": w["mix_norm"][1], "c_w_in": w["c_w_in"], "c_lb_logits": w["c_lb_logits"], "c_w_out": w["c_w_out"],
                 "ogain": cst["ogain"], "bdmask": cst["bdmask"], "rowmask": cst["rowmask"], "flag": cst["flag"]}
        scr = {"oloc": oloc, "qhat": qhat, "sfin": sfin.rearrange("(h d) e -> h d e", d=128)}
        f1 = hgrn_pass1(P, C, xo, h_aps, scr, f, "h1")
        ccsem = P.sem("cc")
        t_cc = P.op("pool", lambda: nc.gpsimd.collective_compute("AllGather", ALU.bypass, replica_groups=groups,
                                                                 ins=[sfin[:, :]], outs=[sall[:, :]]), waits=f1, inc=ccsem)
        sprev = sall[0:4096, :].rearrange("(h d) e -> h d e", d=128)
        f = hgrn_pass2(P, C, xres, 0, h_aps, scr, sprev, list(f1) + [t_cc], "h2")
        f = ffn_stage(P, C, xo, out, 1024, w["ffn2_norm"][1], w["ffn2_w1"][1], w["ffn2_w3"][1], w["ffn2_w2"][1], f, "fb1", nch=nch)
        P.wait("sp", f)
        P.emit()
    return nc


def host_constants():
    j = np.arange(128)[:, None]
    t = np.arange(128)[None, :]
    c = {"ident": np.eye(128, dtype=np.float32),
         "mk_cur": (j <= t).astype(np.float32),
         "mk_prev": (j > t).astype(np.float32),
         "bdmask": ((j // CL == t // CL) & (j <= t)).astype(np.float32),
         "rowmask": (np.arange(128)[:, None] // 32 == np.arange(4)[None, :]).astype(np.float32)}
    return c


def make_in_maps(inputs, n_cores=N_CORES):
    x = np.asarray(inputs["x"], dtype=np.float32)
    hc = host_constants()
    shared = {}
    for k, v in inputs.items():
        if k in ("x", "ab_q_norm", "ab_k_norm", "ab_sinks", "c_o_norm"):
            continue
        v = np.asarray(v, dtype=np.float32)
        if k in ("ffn1_w1", "ffn1_w3", "ffn2_w1", "ffn2_w3"):
            v = tile_in(v)
        elif k in ("ffn1_w2", "ffn2_w2"):
            v = tile_w2(v)
        elif k in ("ab_w_in", "c_w_in"):
            v = tile_in(v[0])
        elif k in ("ab_w_out", "c_w_out"):
            v = tile_out(v[0])
        shared[k] = np.ascontiguousarray(v)
    shared["qg"] = np.asarray(inputs["ab_q_norm"], np.float32).reshape(64, 1)
    shared["kg"] = np.asarray(inputs["ab_k_norm"], np.float32).reshape(64, 1)
    shared["ogain"] = np.asarray(inputs["c_o_norm"], np.float32).reshape(128, 1)
    shared["sinks_l"] = np.ascontiguousarray(np.repeat(np.asarray(inputs["ab_sinks"], np.float32).reshape(16, 2), 64, axis=1).T)
    for k in ("ident", "mk_cur", "mk_prev", "bdmask", "rowmask"):
        shared[k] = hc[k]
    maps = []
    for c in range(n_cores):
        b, half = c // 2, c % 2
        xin = np.ascontiguousarray(x[b, half * 1024:(half + 1) * 1024])
        m = dict(shared)
        m["xin"] = xin
        m["mk_prev0"] = hc["mk_prev"] if half == 1 else np.zeros((128, 128), np.float32)
        m["flag"] = np.full((128, 1), float(half), np.float32)
        maps.append(m)
    return maps


_PROG = {}


def kernel(**inputs):
    ff = int(np.asarray(inputs["ffn1_w1"]).shape[-1])
    n_cores = 2 * int(np.asarray(inputs["x"]).shape[0])
    key = (ff, n_cores)
    if key not in _PROG:
        _PROG[key] = build_program(ff, n_cores)
    nc = _PROG[key]
    maps = make_in_maps(inputs, n_cores)
    res = run_bass_kernel_spmd(nc, maps, core_ids=list(range(n_cores)))
    B = n_cores // 2
    out = np.empty((B, 2048, D), np.float32)
    for c in range(n_cores):
        out[c // 2, (c % 2) * 1024:(c % 2 + 1) * 1024] = res.results[c]["out"]
    return out
```

```python
import numpy as np
import concourse.bass as bass
import concourse.mybir as mybir
from concourse.bass_utils import run_bass_kernel_spmd

F32 = mybir.dt.float32
BF16 = mybir.dt.bfloat16
AF = mybir.ActivationFunctionType
ALU = mybir.AluOpType
AX = mybir.AxisListType

D = 4096
FF = 11008
KT = D // 128
EPS = 1e-6
ENGS = ("pe", "act", "dve", "pool", "sp")


class Sem:
    uid = 0

    def __init__(self, nc, name):
        self.h = nc.alloc_semaphore(name)
        self.n = 0
        Sem.uid += 1
        self.uid = Sem.uid


class Prog:
    def __init__(self, nc):
        self.nc = nc
        self.ops = {e: [] for e in ENGS}
        self.waited = {}
        self.nsem = 0
        self._shared = {}

    def sem(self, name):
        self.nsem += 1
        return Sem(self.nc, f"{name}_{self.nsem}")

    def shared(self, name):
        if name not in self._shared:
            self._shared[name] = self.sem(name)
        return self._shared[name]

    def op(self, eng, fn, waits=(), inc=None, k=1):
        ws = []
        for t in waits:
            if t is None:
                continue
            s, v = t
            key = (eng, s.uid)
            if self.waited.get(key, 0) >= v:
                continue
            self.waited[key] = v
            ws.append((s.h, v))
        tick = None
        if inc is not None:
            inc.n += k
            tick = (inc, inc.n)
        self.ops[eng].append((fn, ws, (inc.h, k) if inc is not None else None))
        return tick

    def dma(self, eng, out, in_, waits=(), inc=None):
        nc = self.nc
        q = {"sp": nc.sync, "pool": nc.gpsimd, "act": nc.scalar}[eng]
        return self.op(eng, lambda: q.dma_start(out=out, in_=in_), waits, inc, 16)

    def wait(self, eng, tickets):
        self.op(eng, None, waits=tickets)

    def emit(self):
        nc = self.nc

        def run(engine, lst, attach):
            for fn, ws, inc in lst:
                if fn is None or not attach or not ws:
                    for h, v in ws:
                        engine.wait_ge(h, v)
                    ws = []
                else:
                    for h, v in ws[:-1]:
                        engine.wait_ge(h, v)
                    ws = ws[-1:]
                if fn is None:
                    continue
                ins = fn()
                for h, v in ws:
                    ins._wait_ge(h, v)
                if inc is not None:
                    ins.then_inc(inc[0], inc[1])

        with nc.Block() as block:
            @block.tensor
            def _(e):
                run(e, self.ops["pe"], False)

            @block.scalar
            def _(e):
                run(e, self.ops["act"], True)

            @block.vector
            def _(e):
                run(e, self.ops["dve"], True)

            @block.gpsimd
            def _(e):
                run(e, self.ops["pool"], True)

            @block.sync
            def _(e):
                run(e, self.ops["sp"], True)


def ntiles_of(T):
    out = []
    s = 0
    while s < T:
        n = min(512, T - s)
        out.append((s, n))
        s += n
    return out


class PsumSets:
    def __init__(self, P, ps):
        self.P = P
        self.ps = ps
        self.free = [None, None]
        self.u = 0

    def next(self):
        s = self.u % 2
        self.u += 1
        return s, self.free[s]

    def release(self, s, ticket):
        self.free[s] = ticket


def load_colvec(P, ctx, vec_ap, n, ident, pe_done, dve_done, pss, name, sp_sem, waits=()):
    nc = P.nc
    rows = ctx.enter_context(nc.sbuf_tensor(name + "_r", [n, 128], F32))
    colv = ctx.enter_context(nc.sbuf_tensor(name + "_c", [128, n], F32))
    t_ld = P.dma("sp", rows[:], vec_ap.rearrange("(k p) -> k p", p=128), waits=list(waits), inc=sp_sem)
    s, fr = pss.next()
    t_pe = P.op("pe", lambda: nc.tensor.transpose(out=pss.ps[:, 4 * s, 0:n], in_=rows[:], identity=ident[0:n, 0:n]),
                waits=[t_ld, fr], inc=pe_done)
    t_cp = P.op("dve", lambda: nc.vector.tensor_copy(out=colv[:], in_=pss.ps[:, 4 * s, 0:n]), waits=[t_pe], inc=dve_done)
    pss.release(s, t_cp)
    return colv, t_cp


def rmsnorm_T(P, ctx, pss, x_src, T, gain_ap, ident, xnT, x_ready, name):
    nc = P.nc
    ntt = T // 128
    pe_done = P.shared("norm_pe")
    dve_done = P.shared("norm_dve")
    act_done = P.shared("norm_act")
    ldsem = [P.shared("norm_ld0"), P.shared("norm_ld1")]
    gsem = P.shared("norm_g")
    from contextlib import ExitStack
    with ExitStack() as sctx:
        gcol, t_g = load_colvec(P, sctx, gain_ap, KT, ident, pe_done, dve_done, pss, name + "gv", gsem, waits=x_ready)
        xt = sctx.enter_context(nc.sbuf_tensor(name + "xt", [128, 2, D], F32))
        junk = sctx.enter_context(nc.sbuf_tensor(name + "junk", [128, D], BF16))
        ss = sctx.enter_context(nc.sbuf_tensor(name + "ss", [128, ntt], F32))
        rstd = sctx.enter_context(nc.sbuf_tensor(name + "rstd", [128, ntt], F32))
        epsc = sctx.enter_context(nc.sbuf_tensor(name + "epsc", [128, 1], F32))
        t_eps = P.op("dve", lambda: nc.vector.memset(epsc[:], EPS), waits=list(x_ready), inc=dve_done)
        xt_free = [None, None]
        junk_free = None
        last = None
        for tt in range(ntt):
            sl = tt % 2
            t_ld = P.dma("sp", xt[:, sl, :], x_src[tt * 128:(tt + 1) * 128, :], waits=list(x_ready) + [xt_free[sl]], inc=ldsem[sl])
            t_sq = P.op("act", lambda sl=sl, tt=tt: nc.scalar.activation(out=junk[:], in_=xt[:, sl, :], func=AF.Square,
                                                                         accum_out=ss[:, tt:tt + 1]),
                        waits=[t_ld, junk_free], inc=act_done)
            junk_free = t_sq
            t_r1 = P.op("act", lambda tt=tt: nc.scalar.activation(out=rstd[:, tt:tt + 1], in_=ss[:, tt:tt + 1], func=AF.Sqrt,
                                                                  scale=1.0 / D, bias=epsc[:, 0:1]),
                        waits=[t_sq, t_eps], inc=act_done)
            t_r2 = P.op("dve", lambda tt=tt: nc.vector.reciprocal(out=rstd[:, tt:tt + 1], in_=rstd[:, tt:tt + 1]),
                        waits=[t_r1], inc=dve_done)
            t_sc = P.op("act", lambda sl=sl, tt=tt: nc.scalar.activation(out=xt[:, sl, :], in_=xt[:, sl, :], func=AF.Copy,
                                                                         scale=rstd[:, tt:tt + 1]),
                        waits=[t_r2], inc=act_done)
            for h in range(2):
                s, fr = pss.next()
                t_pe = None
                for j in range(16):
                    k = h * 16 + j
                    t_pe = P.op("pe", lambda sl=sl, k=k, s=s, j=j: nc.tensor.transpose(
                        out=pss.ps[:, 4 * s + j // 4, (j % 4) * 128:(j % 4 + 1) * 128],
                        in_=xt[:, sl, k * 128:(k + 1) * 128], identity=ident[:, :]),
                        waits=[t_sc, fr], inc=pe_done if j == 15 else None)
                t_ev = None
                for b in range(4):
                    k0 = h * 16 + b * 4
                    t_ev = P.op("dve", lambda s=s, b=b, k0=k0, tt=tt: nc.vector.tensor_tensor(
                        out=xnT[:, k0:k0 + 4, tt * 128:(tt + 1) * 128],
                        in0=pss.ps[:, 4 * s + b, :].rearrange("p (j t) -> p j t", j=4),
                        in1=gcol[:, k0:k0 + 4].unsqueeze(2).to_broadcast([128, 4, 128]), op=ALU.mult),
                        waits=[t_pe, t_g], inc=dve_done)
                pss.release(s, t_ev)
                last = t_ev
            xt_free[sl] = t_pe
    return last


class WeightStream:
    def __init__(self, P, name, tensor, nslots, sname=None):
        self.P = P
        self.t = tensor
        self.n = nslots
        self.ld = [P.shared(f"{sname or name}ld{i}") for i in range(nslots)]
        self.free = [None] * nslots
        self.i = 0
        self.extra = []

    def load(self, dst_fn, src):
        sl = self.i % self.n
        ex = self.extra if self.i < self.n else []
        self.i += 1
        t = self.P.dma("pool", dst_fn(self.t, sl), src, waits=[self.free[sl]] + list(ex), inc=self.ld[sl])
        return sl, t

    def release(self, sl, ticket):
        self.free[sl] = ticket


class Ctx:
    pass


def make_ctx(P, stack, ident_ap, GMAX=14, TMAX=1152):
    nc = P.nc
    C = Ctx()
    C.GMAX = GMAX
    C.ps = stack.enter_context(nc.psum_tensor("ps", [128, 8, 512], F32))
    C.pss = PsumSets(P, C.ps)
    C.ident = stack.enter_context(nc.sbuf_tensor("ident_sb", [128, 128], F32))
    C.xnT = stack.enter_context(nc.sbuf_tensor("xnT", [128, KT, TMAX], BF16))
    C.w13 = stack.enter_context(nc.sbuf_tensor("w13", [128, 3, KT, 256], BF16))
    C.ws13 = WeightStream(P, "ws13", C.w13, 3)
    isem = P.sem("identld")
    t_i = P.dma("sp", C.ident[:], ident_ap, inc=isem)
    C.identb = stack.enter_context(nc.sbuf_tensor("identb_sb", [128, 128], BF16))
    C.ones128 = stack.enter_context(nc.sbuf_tensor("ones128_sb", [128, 128], F32))
    C.epsc = stack.enter_context(nc.sbuf_tensor("epsc_sb", [128, 1], F32))
    csem = P.sem("ctxdve")
    P.op("dve", lambda: nc.vector.memset(C.ones128[:], 1.0))
    P.op("dve", lambda: nc.vector.memset(C.epsc[:], EPS))
    C.t_ident = P.op("dve", lambda: nc.vector.tensor_copy(out=C.identb[:], in_=C.ident[:]), waits=[t_i], inc=csem)
    return C


def ffn_stage(P, C, x_src, x_dst, T, gain_ap, W1, W3, W2, x_ready, name, nch=FF // 128):
    nc = P.nc
    from contextlib import ExitStack
    pss = C.pss
    nts = ntiles_of(T)
    ntt = T // 128
    NP = nch // 2
    assert nch % 2 == 0
    gp_max = C.GMAX // 2 if T <= 1024 else (C.GMAX - 2) // 2
    ngrp = -(-NP // gp_max)
    base = NP // ngrp
    gsz = [base + (1 if i < NP - base * ngrp else 0) for i in range(ngrp)]
    tsets = [list(range(i, min(i + 4, ntt))) for i in range(0, ntt, 4)]
    xnT = C.xnT
    w13 = C.w13

    with ExitStack() as ctx:
        pe_u = P.sem(name + "peu")
        act_d = P.sem(name + "actd")
        dve_d = P.sem(name + "dved")
        xs_ld = [P.shared(f"xs_ld{i}") for i in range(3)]
        xs_st = [P.shared(f"xs_st{i}") for i in range(3)]

        gmax = 2 * max(gsz)
        w2b = ctx.enter_context(nc.sbuf_tensor(name + "w2b", [128, 2, gmax, 512], BF16))
        ws2 = WeightStream(P, name + "ws2", w2b, 2, sname="ws2")
        ws2.extra = list(x_ready)
        t_norm = rmsnorm_T(P, ctx, pss, x_src, T, gain_ap, C.ident, xnT, x_ready, name + "n")

        gT = ctx.enter_context(nc.sbuf_tensor(name + "gT", [128, gmax, T], BF16))
        tmp = ctx.enter_context(nc.sbuf_tensor(name + "tmp", [128, 2, T], BF16))
        xs = ctx.enter_context(nc.sbuf_tensor(name + "xs", [128, 3, 4, 512], F32))

        st_tick = {}
        xs_free = [None, None, None]
        xs_state = {"i": 0}

        def issue_xload(g, n, si):
            ts = tsets[si]
            xi = xs_state["i"]
            xs_state["i"] += 1
            xsl = xi % 3
            r0 = ts[0] * 128
            nr = len(ts)
            src_t = x_src if g == 0 else x_dst
            src = src_t[r0:r0 + nr * 128, n * 512:(n + 1) * 512].rearrange("(j p) c -> p j c", p=128)
            t_ld = P.dma("sp", xs[:, xsl, 0:nr, :], src,
                         waits=[xs_free[xsl], st_tick.get((si, n))] + (list(x_ready) + [t_norm] if g == 0 else []), inc=xs_ld[xsl])
            return xsl, t_ld

        tmp_free = [None, None]
        final = []
        prefetched = {}
        p0 = 0
        for g in range(ngrp):
            np_ = gsz[g]
            pp = p0
            p0 += np_
            G = 2 * np_
            t_gT = None
            for pi in range(np_):
                p = pp + pi
                wl = []
                for which in range(2):
                    if (p, which) in prefetched:
                        wl.append(prefetched.pop((p, which)))
                    else:
                        wl.append(C.ws13.load(lambda t, sl: t[:, sl], (W1 if which == 0 else W3)[p]))
                for ci in range(2):
                    gi = 2 * pi + ci
                    tsl = gi % 2
                    t_act = None
                    for which in range(2):
                        slw, t_w = wl[which]
                        s, fr = pss.next()
                        t_pe = None
                        for k in range(KT):
                            for bi, (n0, nn) in enumerate(nts):
                                lastmm = (k == KT - 1 and bi == len(nts) - 1)
                                t_pe = P.op("pe", lambda slw=slw, k=k, ci=ci, s=s, bi=bi, n0=n0, nn=nn: nc.tensor.matmul(
                                    pss.ps[:, 4 * s + bi, 0:nn], lhsT=w13[:, slw, k, ci * 128:(ci + 1) * 128],
                                    rhs=xnT[:, k, n0:n0 + nn], start=(k == 0), stop=(k == KT - 1)),
                                    waits=[t_w, fr, t_norm], inc=pe_u if lastmm else None)
                        if ci == 1:
                            C.ws13.release(slw, t_pe)
                        psv = pss.ps[:, 4 * s:4 * s + 4, :].rearrange("p b n -> p (b n)")[:, 0:T]
                        if which == 0:
                            t_act = P.op("act", lambda psv=psv, tsl=tsl: nc.scalar.activation(out=tmp[:, tsl, :], in_=psv, func=AF.Silu),
                                         waits=[t_pe, tmp_free[tsl]], inc=act_d)
                            pss.release(s, t_act)
                        else:
                            t_d = P.op("dve", lambda psv=psv, tsl=tsl, gi=gi: nc.vector.tensor_tensor(
                                out=gT[:, gi, :], in0=psv, in1=tmp[:, tsl, :], op=ALU.mult),
                                waits=[t_pe, t_act], inc=dve_d)
                            pss.release(s, t_d)
                            tmp_free[tsl] = t_d
                            t_gT = t_d
            units = [(n, si) for n in range(8) for si in range(len(tsets))]
            pre = issue_xload(g, units[0][0], units[0][1])
            wcur = None
            for ui, (n, si) in enumerate(units):
                ts = tsets[si]
                nr = len(ts)
                if si == 0:
                    src = W2[n][:, 2 * pp:2 * pp + G, :]
                    wcur = ws2.load(lambda t, sl, G=G: t[:, sl, 0:G, :], src)
                    if n == 2 and g + 1 < ngrp:
                        pn = pp + np_
                        for which in range(2):
                            prefetched[(pn, which)] = C.ws13.load(lambda t, sl: t[:, sl], (W1 if which == 0 else W3)[pn])
                        if gsz[g + 1] > 1:
                            prefetched[(pn + 1, 0)] = C.ws13.load(lambda t, sl: t[:, sl], W1[pn + 1])
                slw, t_w = wcur
                xsl, t_ld = pre
                if ui + 1 < len(units):
                    pre = issue_xload(g, units[ui + 1][0], units[ui + 1][1])
                s, fr = pss.next()
                t_pe = None
                for c in range(G):
                    for j, tt in enumerate(ts):
                        lastmm = (c == G - 1 and j == nr - 1)
                        t_pe = P.op("pe", lambda c=c, tt=tt, s=s, j=j, slw=slw, G=G: nc.tensor.matmul(
                            pss.ps[:, 4 * s + j, :], lhsT=gT[:, c, tt * 128:(tt + 1) * 128], rhs=w2b[:, slw, c, :],
                            start=(c == 0), stop=(c == G - 1)),
                            waits=[t_w, fr, t_gT], inc=pe_u if lastmm else None)
                if si == len(tsets) - 1:
                    ws2.release(slw, t_pe)
                t_ev = None
                for j in range(nr):
                    t_ev = P.op("dve", lambda s=s, j=j, xsl=xsl: nc.vector.scalar_tensor_tensor(
                        out=xs[:, xsl, j, :], in0=pss.ps[:, 4 * s + j, :], scalar=0.5, in1=xs[:, xsl, j, :],
                        op0=ALU.mult, op1=ALU.add),
                        waits=[t_pe, t_ld], inc=dve_d)
                pss.release(s, t_ev)
                r0 = ts[0] * 128
                dst = x_dst[r0:r0 + nr * 128, n * 512:(n + 1) * 512].rearrange("(j p) c -> p j c", p=128)
                t_st = P.dma("sp", dst, xs[:, xsl, 0:nr, :], waits=[t_ev], inc=xs_st[xsl])
                xs_free[xsl] = t_st
                st_tick[(si, n)] = t_st
                if g == ngrp - 1:
                    final.append(t_st)
    return final


def accum_rows(P, C, name, actT, G, load_w, x, row0, scale, first_waits, st_tick, dve_d, pe_u, sctx):
    nc = P.nc
    pss = C.pss
    xs = sctx.enter_context(nc.sbuf_tensor(name + "xs", [128, 3, 4, 512], F32))
    xs_ld = [P.shared(f"xs_ld{i}") for i in range(3)]
    xs_st = [P.shared(f"xs_st{i}") for i in range(3)]
    xs_free = [None, None, None]
    tsets = [[0, 1, 2, 3], [4, 5, 6, 7]]
    units = [(n, si) for n in range(8) for si in range(2)]
    state = {"i": 0}

    def xload(n, si):
        xi = state["i"]
        state["i"] += 1
        xsl = xi % 3
        r0 = row0 + tsets[si][0] * 128
        src = x[r0:r0 + 512, n * 512:(n + 1) * 512].rearrange("(j p) c -> p j c", p=128)
        t = P.dma("sp", xs[:, xsl, :, :], src, waits=[xs_free[xsl], st_tick.get((si, n))] + list(first_waits), inc=xs_ld[xsl])
        return xsl, t

    pre = xload(*units[0])
    final = []
    wcur = None
    for ui, (n, si) in enumerate(units):
        ts = tsets[si]
        if si == 0:
            wcur = load_w(n)
        wfn, t_w, rel = wcur
        xsl, t_ld = pre
        if ui + 1 < len(units):
            pre = xload(*units[ui + 1])
        s, fr = pss.next()
        t_pe = None
        for c in range(G):
            for j, tt in enumerate(ts):
                lastmm = (c == G - 1 and j == 3)
                t_pe = P.op("pe", lambda c=c, tt=tt, s=s, j=j, wfn=wfn: nc.tensor.matmul(
                    pss.ps[:, 4 * s + j, :], lhsT=actT[:, c, tt * 128:(tt + 1) * 128], rhs=wfn(c),
                    start=(c == 0), stop=(c == G - 1)),
                    waits=[t_w, fr] + list(first_waits), inc=pe_u if lastmm else None)
        if si == 1:
            rel(t_pe)
        t_ev = None
        for j in range(4):
            t_ev = P.op("dve", lambda s=s, j=j, xsl=xsl: nc.vector.scalar_tensor_tensor(
                out=xs[:, xsl, j, :], in0=pss.ps[:, 4 * s + j, :], scalar=scale, in1=xs[:, xsl, j, :],
                op0=ALU.mult, op1=ALU.add),
                waits=[t_pe, t_ld], inc=dve_d)
        pss.release(s, t_ev)
        r0 = row0 + ts[0] * 128
        dst = x[r0:r0 + 512, n * 512:(n + 1) * 512].rearrange("(j p) c -> p j c", p=128)
        t_st = P.dma("sp", dst, xs[:, xsl, :, :], waits=[t_ev], inc=xs_st[xsl])
        xs_free[xsl] = t_st
        st_tick[(si, n)] = t_st
        final.append(t_st)
    return final


def ab_stage(P, C, x, aps, x_ready, name, halo, groups, dbg=None):
    from contextlib import ExitStack
    nc = P.nc
    pss = C.pss
    xnT = C.xnT
    w13 = C.w13
    T, TO = 1152, 1024
    Winr = aps["ab_w_in"]
    Wout = aps["ab_w_out"]
    nts_o = ntiles_of(TO)

    with ExitStack() as ctx:
        pe_u = P.sem(name + "peu")
        act_d = P.sem(name + "actd")
        dve_d = P.sem(name + "dved")
        csem = P.sem(name + "csem")
        t_norm = rmsnorm_T(P, ctx, pss, x, TO, aps["mix_norm"], C.ident, xnT, x_ready, name + "n")
        W0 = [t_norm]

        def sb(nm, shape, dt):
            return ctx.enter_context(nc.sbuf_tensor(name + nm, shape, dt))

        qg = sb("qg", [64, 1], F32)
        kg = sb("kg", [64, 1], F32)
        esink = sb("esink", [128, 16], F32)
        mk_cur = sb("mkc", [128, 128], F32)
        mk_prev = sb("mkp", [128, 128], F32)
        mk_prev0 = sb("mkp0", [128, 128], F32)
        ones64 = sb("ones64", [64, 64], F32)
        onespad = sb("onespad", [128, 192], BF16)
        epsc = sb("epsc", [128, 1], F32)
        outT = sb("outT", [128, 16, TO], BF16)

        t_c = None
        for dst, srcap in ((qg[:], aps["qg"]), (kg[:], aps["kg"]), (esink[:], aps["sinks_l"]), (mk_cur[:], aps["mk_cur"]),
                           (mk_prev[:], aps["mk_prev"]), (mk_prev0[:], aps["mk_prev0"])):
            t_c = P.dma("sp", dst, srcap, waits=W0, inc=csem)
        t_es = P.op("act", lambda: nc.scalar.activation(out=esink[:], in_=esink[:], func=AF.Exp), waits=[t_c], inc=act_d)
        P.op("dve", lambda: nc.vector.memset(ones64[:], 1.0), waits=W0)
        P.op("dve", lambda: nc.vector.memset(onespad[:], 0.0), waits=W0)
        P.op("dve", lambda: nc.vector.memset(onespad[:, 64:128], 1.0))
        t_ms = P.op("dve", lambda: nc.vector.memset(epsc[:], EPS), inc=dve_d)

        def slab_load(j):
            return C.ws13.load(lambda t, sl: t[:, sl], Winr[j])

        st_tick = {}

        with ExitStack() as actx:
            def sba(nm, shape, dt):
                return actx.enter_context(nc.sbuf_tensor(name + nm, shape, dt))
            kT = sba("kT", [64, 4, T], BF16)
            Vpad = sba("Vpad", [128, 4, 9, 192], BF16)
            qT = sba("qT", [64, 2, 4, TO], BF16)
            sqb = sba("sqb", [64, TO], F32)
            qraw = sba("qraw", [64, TO], F32)
            E = sba("E", [128, 2, 2, 512], BF16)
            rd = sba("rd", [128, 1, 2, 128], F32)
            t_vz = P.op("dve", lambda: nc.vector.memset(Vpad[:], 0.0), waits=W0, inc=dve_d)
            st = {"sqb_free": None}

            def head_proj_norm(slw, t_w, h4, tok0, ntok, gcol, dst_ap, extra_w=()):
                nts = ntiles_of(ntok)
                s, fr = pss.next()
                t_pe = None
                for k in range(KT):
                    for bi, (n0, nn) in enumerate(nts):
                        lastmm = (k == KT - 1 and bi == len(nts) - 1)
                        t_pe = P.op("pe", lambda k=k, bi=bi, n0=n0, nn=nn, s=s: nc.tensor.matmul(
                            pss.ps[0:64, 4 * s + bi, 0:nn], lhsT=w13[:, slw, k, h4 * 64:(h4 + 1) * 64],
                            rhs=xnT[:, k, tok0 + n0:tok0 + n0 + nn], start=(k == 0), stop=(k == KT - 1)),
                            waits=[t_w, fr, t_norm], inc=pe_u if lastmm else None)
                qps = pss.ps[0:64, 4 * s:4 * s + 4, :].rearrange("p b n -> p (b n)")[:, 0:ntok]
                t_cp = P.op("dve", lambda: nc.vector.tensor_copy(out=qraw[:, 0:ntok], in_=qps), waits=[t_pe, st["sqb_free"]], inc=dve_d)
                pss.release(s, t_cp)
                t_sq = P.op("act", lambda: nc.scalar.activation(out=sqb[:, 0:ntok], in_=qraw[:, 0:ntok], func=AF.Square),
                            waits=[t_cp], inc=act_d)
                s2, fr2 = pss.next()
                t_pe2 = None
                for bi, (n0, nn) in enumerate(nts):
                    t_pe2 = P.op("pe", lambda bi=bi, n0=n0, nn=nn: nc.tensor.matmul(
                        pss.ps[0:64, 4 * s2 + bi, 0:nn], lhsT=ones64[:, :], rhs=sqb[:, n0:n0 + nn], start=True, stop=True),
                        waits=[t_sq, fr2, t_ms], inc=pe_u if bi == len(nts) - 1 else None)
                ssps = pss.ps[0:64, 4 * s2:4 * s2 + 4, :].rearrange("p b n -> p (b n)")[:, 0:ntok]
                t_rt = P.op("act", lambda: nc.scalar.activation(out=sqb[:, 0:ntok], in_=ssps, func=AF.Sqrt,
                                                                scale=1.0 / 64, bias=epsc[0:64, 0:1]),
                            waits=[t_pe2, t_ms], inc=act_d)
                pss.release(s2, t_rt)
                t_rc = P.op("dve", lambda: nc.vector.reciprocal(out=sqb[:, 0:ntok], in_=sqb[:, 0:ntok]), waits=[t_rt], inc=dve_d)
                t_q = P.op("dve", lambda: nc.vector.scalar_tensor_tensor(
                    out=dst_ap, in0=qraw[:, 0:ntok], scalar=gcol[:, 0:1], in1=sqb[:, 0:ntok], op0=ALU.mult, op1=ALU.mult),
                    waits=[t_rc, t_c] + list(extra_w), inc=dve_d)
                st["sqb_free"] = t_q
                return t_q

            slk, t_wk = slab_load(8)
            t_k = None
            for hk in range(4):
                t_k = head_proj_norm(slk, t_wk, hk, 0, TO, kg, kT[:, hk, 128:T])
            slv, t_wv = slab_load(9)
            t_v = None
            blks = list(range(8))
            for u0 in range(0, 8, 4):
                ub = blks[u0:u0 + 4]
                s, fr = pss.next()
                t_pe = None
                for bi, blk in enumerate(ub):
                    for k in range(KT):
                        lastmm = (bi == len(ub) - 1 and k == KT - 1)
                        t_pe = P.op("pe", lambda k=k, bi=bi, blk=blk, s=s: nc.tensor.matmul(
                            pss.ps[:, 4 * s + bi, 0:256], lhsT=xnT[:, k, blk * 128:(blk + 1) * 128], rhs=w13[:, slv, k, :],
                            start=(k == 0), stop=(k == KT - 1)),
                            waits=[t_wv, fr, t_norm], inc=pe_u if lastmm else None)
                for bi, blk in enumerate(ub):
                    t_v = P.op("dve", lambda bi=bi, blk=blk, s=s: nc.vector.tensor_copy(
                        out=Vpad[:, :, blk + 1, 64:128], in_=pss.ps[:, 4 * s + bi, 0:256].rearrange("p (h d) -> p h d", h=4)),
                        waits=[t_pe, t_vz], inc=dve_d)
                pss.release(s, t_v)
                t_pe_v = t_pe
            C.ws13.release(slk, t_pe_v)
            C.ws13.release(slv, t_pe_v)
            q0 = slab_load(0)
            hsem = P.sem(name + "hst")
            hsem2 = P.sem(name + "hld")
            ccs = P.sem(name + "hcc")
            h_out, h_all = halo
            t_hs = P.dma("sp", h_out[0:64, :].rearrange("p (h t) -> p h t", h=4), kT[:, :, TO:T], waits=[t_k], inc=hsem)
            t_hs = P.dma("sp", h_out[64:192, 0:256].rearrange("p (h d) -> p h d", h=4), Vpad[:, :, 8, 64:128], waits=[t_v], inc=hsem)
            t_cc = P.op("pool", lambda: nc.gpsimd.collective_compute("AllGather", ALU.bypass, replica_groups=groups,
                                                                     ins=[h_out[:, :]], outs=[h_all[:, :]]), waits=[t_hs], inc=ccs)
            t_halo = P.dma("sp", kT[:, :, 0:128], h_all[0:64, :].rearrange("p (h t) -> p h t", h=4), waits=[t_cc], inc=hsem2)
            t_halo = P.dma("sp", Vpad[:, :, 0, 64:128], h_all[64:192, 0:256].rearrange("p (h d) -> p h d", h=4), waits=[t_cc, t_vz], inc=hsem2)

            qT_free = [None, None]
            E_free = [None, None]
            rd_free = [None, None]
            cnt = {"e": 0, "r": 0}
            t_last = None
            for j in range(8):
                hk = j // 2
                qb = j % 2
                slq, t_wq = q0 if j == 0 else slab_load(j)
                t_qs = None
                for h4 in range(4):
                    t_qs = head_proj_norm(slq, t_wq, h4, 0, TO, qg, qT[:, qb, h4, :], extra_w=[qT_free[qb]])
                rel_done = {"d": False}

                def scores(n, j=j, hk=hk, qb=qb, t_qs=t_qs, slq=slq, rel_done=rel_done):
                    s, fr = pss.next()
                    eb = cnt["e"] % 2
                    cnt["e"] += 1
                    t_pe = None
                    for kb in range(2):
                        blk = n + kb
                        t_pe = P.op("pe", lambda kb=kb, blk=blk, s=s: nc.tensor.matmul(
                            pss.ps[:, 4 * s + kb, :], lhsT=kT[:, hk, blk * 128:(blk + 1) * 128],
                            rhs=qT[:, qb, :, n * 128:(n + 1) * 128], start=True, stop=True),
                            waits=[fr, t_qs, t_k, t_halo], inc=pe_u if kb == 1 else None)
                    if not rel_done["d"]:
                        C.ws13.release(slq, t_pe)
                        rel_done["d"] = True
                    t_ex = None
                    for kb in range(2):
                        t_ex = P.op("act", lambda kb=kb, s=s, eb=eb: nc.scalar.activation(
                            out=E[:, eb, kb, :], in_=pss.ps[:, 4 * s + kb, :], func=AF.Exp, scale=0.125),
                            waits=[t_pe, E_free[eb]], inc=act_d)
                    pss.release(s, t_ex)
                    t_mk = None
                    for kb in range(2):
                        mk = mk_cur if kb == 1 else (mk_prev0 if n == 0 else mk_prev)
                        t_mk = P.op("dve", lambda kb=kb, eb=eb, mk=mk: nc.vector.tensor_tensor(
                            out=E[:, eb, kb, :].rearrange("p (h q) -> p h q", h=4),
                            in0=E[:, eb, kb, :].rearrange("p (h q) -> p h q", h=4),
                            in1=mk[:, :].unsqueeze(1).to_broadcast([128, 4, 128]), op=ALU.mult),
                            waits=[t_ex, t_c], inc=dve_d)
                    return (n, eb, t_mk, t_pe)

                def pv(sc, j=j, hk=hk):
                    n, eb, t_mk, t_sc = sc
                    s2, fr2 = pss.next()
                    rb = 0
                    t_pv = None
                    for pr in range(2):
                        for bank in range(2):
                            c = 0
                            for hh in range(2):
                                for kb in range(2):
                                    lo = 64 - 64 * hh
                                    lhs = Vpad[:, hk, n + kb, lo:lo + 128] if bank == 0 else onespad[:, lo:lo + 128]
                                    lastmm = (pr == 1 and bank == 1 and c == 3)
                                    t_pv = P.op("pe", lambda lhs=lhs, kb=kb, pr=pr, hh=hh, bank=bank, c=c, s2=s2, eb=eb: nc.tensor.matmul(
                                        pss.ps[:, 4 * s2 + bank, pr * 128:(pr + 1) * 128], lhsT=lhs,
                                        rhs=E[:, eb, kb, (pr * 2 + hh) * 128:(pr * 2 + hh + 1) * 128],
                                        start=(c == 0), stop=(c == 3)),
                                        waits=[t_mk, fr2, t_v], inc=pe_u if lastmm else None)
                                    c += 1
                    E_free[eb] = t_pv
                    t_a = None
                    for pr in range(2):
                        t_a = P.op("dve", lambda pr=pr, rb=rb, s2=s2: nc.vector.tensor_scalar(
                            out=rd[:, rb, pr, :], in0=pss.ps[:, 4 * s2 + 1, pr * 128:(pr + 1) * 128],
                            scalar1=esink[:, 2 * j + pr:2 * j + pr + 1], scalar2=None, op0=ALU.add),
                            waits=[t_pv, rd_free[rb], t_es], inc=dve_d)
                    t_r = P.op("dve", lambda rb=rb: nc.vector.reciprocal(out=rd[:, rb], in_=rd[:, rb]), waits=[t_a], inc=dve_d)
                    t_o = P.op("dve", lambda rb=rb, s2=s2, n=n: nc.vector.tensor_tensor(
                        out=outT[:, 2 * j:2 * j + 2, n * 128:(n + 1) * 128],
                        in0=pss.ps[:, 4 * s2, 0:256].rearrange("p (a q) -> p a q", a=2), in1=rd[:, rb], op=ALU.mult),
                        waits=[t_r], inc=dve_d)
                    pss.release(s2, t_o)
                    rd_free[rb] = t_o
                    return t_o, t_sc

                prev = None
                for n in range(8):
                    cur = scores(n)
                    if prev is not None:
                        t_last, _ = pv(prev)
                    prev = cur
                t_last, t_sc_last = pv(prev)
                qT_free[qb] = t_sc_last
        with ExitStack() as octx:
            def load_wo(n, part=0):
                src = Wout[part][n]
                sl, t = C.ws13.load(lambda t_, sl_: t_[:, sl_].rearrange("p k f -> p (k f)").rearrange("p (k f) -> p k f", k=16), src)
                wv = w13[:, sl].rearrange("p k f -> p (k f)").rearrange("p (k f) -> p k f", k=16)
                return (lambda c: wv[:, c, :]), t, (lambda tk, sl=sl: C.ws13.release(sl, tk))
            if dbg is not None:
                dsem = P.sem(name + "dbga")
                t_last = P.dma("sp", dbg["outA"], outT[:], waits=[t_last], inc=dsem)
            fin_a = accum_rows(P, C, name + "oa", outT, 16, load_wo, x, 0, 1.0, [t_last], st_tick, dve_d, pe_u, octx)

        with ExitStack() as gctx:
            def sbg(nm, shape, dt):
                return gctx.enter_context(nc.sbuf_tensor(name + nm, shape, dt))
            G0 = [fin_a[-1]] + fin_a
            wraw = sbg("wraw", [128, 16, 128], F32)
            wsT = sbg("wsT", [128, 16, 128], BF16)
            bias = sbg("bias", [128, 16, 128], F32)
            lng = sbg("lng", [128, 2, 256], F32)
            lnb = sbg("lnb", [128, 2, 256], F32)
            uT = sbg("uT", [128, 2, TO], F32)
            vf = sbg("vf", [128, 2, 256], F32)
            vsq = sbg("vsq", [128, 256], F32)
            stt = sbg("stt", [128, 2, 8], F32)
            vn = sbg("vn", [128, 8, 256], BF16)
            tmpf = sbg("tmpf", [128, 2, 512], F32)
            gsem = P.sem(name + "gsem")
            lnsem = [P.sem(name + "lns0"), P.sem(name + "lns1")]
            t_g1 = P.dma("sp", wraw[:], aps["ab_w_s"].rearrange("g t s -> t g s"), waits=G0, inc=gsem)
            t_g1 = P.dma("sp", bias[:].rearrange("p g t -> p (g t)"), aps["ab_b_s"].rearrange("g t -> (g t)").partition_broadcast(128),
                         waits=G0, inc=gsem)
            t_ws = None
            for u0 in range(0, 16, 16):
                for q4 in range(4):
                    s, fr = pss.next()
                    t_pe = None
                    for b in range(4):
                        g = q4 * 4 + b
                        t_pe = P.op("pe", lambda g=g, b=b, s=s: nc.tensor.transpose(
                            out=pss.ps[:, 4 * s, b * 128:(b + 1) * 128], in_=wraw[:, g, :], identity=C.ident[:, :]),
                            waits=[t_g1, fr], inc=pe_u if b == 3 else None)
                    t_ws = P.op("dve", lambda q4=q4, s=s: nc.vector.tensor_tensor(
                        out=wsT[:, q4 * 4:q4 * 4 + 4, :], in0=pss.ps[:, 4 * s, :].rearrange("p (g t) -> p g t", g=4),
                        in1=mk_cur[:, :].unsqueeze(1).to_broadcast([128, 4, 128]), op=ALU.mult),
                        waits=[t_pe, t_c], inc=dve_d)
                    pss.release(s, t_ws)
            ln_free = [None, None]
            uT_free = None
            vn_free = None
            vf_free = [None, None]
            tmpf_free = [None, None]
            cntg = {"v": 0, "t": 0}
            t_lastg = None
            for j in range(8):
                lb = j % 2
                t_ln = P.dma("sp", lng[:, lb, :], aps["ab_v_ln_g"][j * 256:(j + 1) * 256].partition_broadcast(128),
                             waits=[ln_free[lb]] + G0, inc=lnsem[lb])
                t_ln = P.dma("sp", lnb[:, lb, :], aps["ab_v_ln_b"][j * 256:(j + 1) * 256].partition_broadcast(128),
                             waits=[ln_free[lb]] + G0, inc=lnsem[lb])
                slu, t_wu = slab_load(10 + j)
                t_u = None
                for gg in range(2):
                    s, fr = pss.next()
                    t_pe = None
                    for k in range(KT):
                        for bi, (n0, nn) in enumerate(nts_o):
                            lastmm = (k == KT - 1 and bi == 1)
                            t_pe = P.op("pe", lambda k=k, bi=bi, n0=n0, nn=nn, s=s, gg=gg, slu=slu: nc.tensor.matmul(
                                pss.ps[:, 4 * s + bi, 0:nn], lhsT=w13[:, slu, k, gg * 128:(gg + 1) * 128],
                                rhs=xnT[:, k, n0:n0 + nn], start=(k == 0), stop=(k == KT - 1)),
                                waits=[t_wu, fr], inc=pe_u if lastmm else None)
                    if gg == 1:
                        C.ws13.release(slu, t_pe)
                    t_u = P.op("act", lambda s=s, gg=gg: nc.scalar.activation(
                        out=uT[:, gg, :], in_=pss.ps[:, 4 * s:4 * s + 2, :].rearrange("p b n -> p (b n)"), func=AF.Gelu),
                        waits=[t_pe, uT_free] + G0, inc=act_d)
                    pss.release(s, t_u)
                slv2, t_wv2 = slab_load(18 + j)
                t_vn = None
                for half in range(2):
                    s, fr = pss.next()
                    t_pe = None
                    for bi in range(4):
                        tt = half * 4 + bi
                        for k in range(KT):
                            lastmm = (bi == 3 and k == KT - 1)
                            t_pe = P.op("pe", lambda k=k, bi=bi, tt=tt, s=s, slv2=slv2: nc.tensor.matmul(
                                pss.ps[:, 4 * s + bi, 0:256], lhsT=xnT[:, k, tt * 128:(tt + 1) * 128],
                                rhs=w13[:, slv2, k, :], start=(k == 0), stop=(k == KT - 1)),
                                waits=[t_wv2, fr], inc=pe_u if lastmm else None)
                    if half == 1:
                        C.ws13.release(slv2, t_pe)
                    t_x = None
                    for bi in range(4):
                        tt = half * 4 + bi
                        vb_ = cntg["v"] % 2
                        cntg["v"] += 1
                        v3 = vf[:, vb_, :].rearrange("p (g c) -> p g c", g=2)
                        t_ge = P.op("act", lambda bi=bi, s=s, vb_=vb_: nc.scalar.activation(
                            out=vf[:, vb_, :], in_=pss.ps[:, 4 * s + bi, 0:256], func=AF.Gelu),
                            waits=[t_pe, vf_free[vb_]] + G0, inc=act_d)
                        t_x = t_ge
                        t1 = P.op("dve", lambda v3=v3, vb_=vb_: nc.vector.tensor_reduce(
                            out=stt[:, vb_, 0:2], in_=v3, axis=AX.X, op=ALU.add), waits=[t_ge], inc=dve_d)
                        t2 = P.op("dve", lambda vb_=vb_: nc.vector.tensor_tensor(
                            out=vsq[:, :], in0=vf[:, vb_, :], in1=vf[:, vb_, :], op=ALU.mult), waits=[t1], inc=dve_d)
                        t3 = P.op("dve", lambda vb_=vb_: nc.vector.tensor_reduce(
                            out=stt[:, vb_, 2:4], in_=vsq[:, :].rearrange("p (g c) -> p g c", g=2), axis=AX.X, op=ALU.add),
                            waits=[t2], inc=dve_d)
                        t4 = P.op("dve", lambda vb_=vb_: nc.vector.tensor_scalar(
                            out=stt[:, vb_, 4:6], in0=stt[:, vb_, 0:2], scalar1=1.0 / 128, scalar2=None, op0=ALU.mult),
                            waits=[t3], inc=dve_d)
                        t5 = P.op("dve", lambda vb_=vb_: nc.vector.tensor_tensor(
                            out=stt[:, vb_, 0:2], in0=stt[:, vb_, 4:6], in1=stt[:, vb_, 4:6], op=ALU.mult), waits=[t4], inc=dve_d)
                        t6 = P.op("dve", lambda vb_=vb_: nc.vector.scalar_tensor_tensor(
                            out=stt[:, vb_, 2:4], in0=stt[:, vb_, 2:4], scalar=1.0 / 128, in1=stt[:, vb_, 0:2],
                            op0=ALU.mult, op1=ALU.subtract), waits=[t5], inc=dve_d)
                        t7 = P.op("act", lambda vb_=vb_: nc.scalar.activation(
                            out=stt[:, vb_, 6:8], in_=stt[:, vb_, 2:4], func=AF.Sqrt, bias=epsc[:, 0:1], scale=1.0),
                            waits=[t6, t_ms], inc=act_d)
                        t8 = P.op("dve", lambda vb_=vb_: nc.vector.reciprocal(out=stt[:, vb_, 6:8], in_=stt[:, vb_, 6:8]),
                                  waits=[t7], inc=dve_d)
                        t9 = None
                        for gg in range(2):
                            t9 = P.op("dve", lambda vb_=vb_, gg=gg: nc.vector.tensor_scalar(
                                out=vf[:, vb_, gg * 128:(gg + 1) * 128], in0=vf[:, vb_, gg * 128:(gg + 1) * 128],
                                scalar1=stt[:, vb_, 4 + gg:5 + gg], scalar2=stt[:, vb_, 6 + gg:7 + gg],
                                op0=ALU.subtract, op1=ALU.mult), waits=[t8], inc=dve_d)
                        t10 = P.op("dve", lambda vb_=vb_, lb=lb: nc.vector.tensor_tensor(
                            out=vf[:, vb_, :], in0=vf[:, vb_, :], in1=lng[:, lb, :], op=ALU.mult), waits=[t9, t_ln], inc=dve_d)
                        t_vn = P.op("dve", lambda vb_=vb_, lb=lb, tt=tt: nc.vector.tensor_tensor(
                            out=vn[:, tt, :], in0=vf[:, vb_, :], in1=lnb[:, lb, :], op=ALU.add),
                            waits=[t10, vn_free], inc=dve_d)
                        vf_free[vb_] = t_vn
                    pss.release(s, t_x)
                ln_free[lb] = t_vn
                t_sp = None
                for gg in range(2):
                    g = 2 * j + gg
                    for half in range(2):
                        s, fr = pss.next()
                        t_pe = None
                        for c4 in range(4):
                            tt = half * 4 + c4
                            t_pe = P.op("pe", lambda tt=tt, c4=c4, s=s, gg=gg, g=g: nc.tensor.matmul(
                                pss.ps[:, 4 * s, c4 * 128:(c4 + 1) * 128], lhsT=vn[:, tt, gg * 128:(gg + 1) * 128],
                                rhs=wsT[:, g, :], start=True, stop=True),
                                waits=[t_vn, t_ws, fr], inc=pe_u if c4 == 3 else None)
                        t_sp = t_pe
                        tb = cntg["t"] % 2
                        cntg["t"] += 1
                        t_b = P.op("dve", lambda s=s, g=g, tb=tb: nc.vector.tensor_tensor(
                            out=tmpf[:, tb, :].rearrange("p (a t) -> p a t", a=4),
                            in0=pss.ps[:, 4 * s, :].rearrange("p (a t) -> p a t", a=4),
                            in1=bias[:, g, :].unsqueeze(1).to_broadcast([128, 4, 128]), op=ALU.add),
                            waits=[t_pe, t_g1, tmpf_free[tb]], inc=dve_d)
                        pss.release(s, t_b)
                        t_lastg = P.op("dve", lambda g=g, gg=gg, half=half, tb=tb: nc.vector.tensor_tensor(
                            out=outT[:, g, half * 512:(half + 1) * 512], in0=tmpf[:, tb, :],
                            in1=uT[:, gg, half * 512:(half + 1) * 512], op=ALU.mult),
                            waits=[t_b, t_u], inc=dve_d)
                        tmpf_free[tb] = t_lastg
                uT_free = t_lastg
                vn_free = t_sp
        with ExitStack() as octx:
            if dbg is not None:
                dsem2 = P.sem(name + "dbgb")
                t_lastg = P.dma("sp", dbg["outB"], outT[:], waits=[t_lastg], inc=dsem2)
            fin_b = accum_rows(P, C, name + "ob", outT, 16, lambda n: load_wo(n, 1), x, 0, 1.0, [t_lastg], st_tick, dve_d, pe_u, octx)
    return fin_b


CL = 32
NCHK = 1024 // CL


def hgrn_consts(P, C, ctx, aps, waits, name):
    nc = P.nc
    pss = C.pss
    pe_d = P.shared("norm_pe")
    dve_d = P.shared("norm_dve")
    act_d = P.shared("norm_act")
    sem = P.shared("norm_g")
    H = Ctx()
    p0, t0 = load_colvec(P, ctx, aps["c_lb_logits"][0], KT, C.ident, pe_d, dve_d, pss, name + "p0", sem, waits=waits)
    sem2 = P.sem(name + "ld2")
    p1, t1 = load_colvec(P, ctx, aps["c_lb_logits"][1], KT, C.ident, pe_d, dve_d, pss, name + "p1", sem2, waits=waits)
    H.lbc = ctx.enter_context(nc.sbuf_tensor(name + "lbc", [128, KT], F32))
    H.oml = ctx.enter_context(nc.sbuf_tensor(name + "oml", [128, KT], F32))
    ta = P.op("dve", lambda: nc.vector.tensor_tensor(out=H.lbc[:], in0=p1[:], in1=p0[:], op=ALU.subtract), waits=[t0, t1], inc=dve_d)
    tb = P.op("act", lambda: nc.scalar.activation(out=H.lbc[:], in_=H.lbc[:], func=AF.Sigmoid), waits=[ta], inc=act_d)
    H.t = P.op("dve", lambda: nc.vector.tensor_scalar(out=H.oml[:], in0=H.lbc[:], scalar1=-1.0, scalar2=1.0, op0=ALU.mult, op1=ALU.add),
               waits=[tb], inc=dve_d)
    return H


def hgrn_pass1(P, C, x, aps, scr, x_ready, name):
    from contextlib import ExitStack
    nc = P.nc
    pss = C.pss
    xnT = C.xnT
    w13 = C.w13
    T = 1024
    nts = ntiles_of(T)
    Winr = aps["c_w_in"]
    psb = C.ps[:].rearrange("p b n -> p (b n)").bitcast(BF16).rearrange("p (b n) -> p b n", b=8)
    with ExitStack() as ctx:
        pe_u = P.sem(name + "peu")
        act_d = P.sem(name + "actd")
        dve_d = P.sem(name + "dved")
        csem = P.sem(name + "csem")
        t_norm = rmsnorm_T(P, ctx, pss, x, T, aps["mix_norm"], C.ident, xnT, x_ready, name + "n")
        W0 = [t_norm]
        H = hgrn_consts(P, C, ctx, aps, W0, name + "hc")

        def sb(nm, shape, dt):
            return ctx.enter_context(nc.sbuf_tensor(name + nm, shape, dt))
        segm = sb("segm", [128, T], BF16)
        onec = sb("onec", [128, 1], F32)
        zeroc = sb("zeroc", [128, 1], F32)
        bdm = sb("bdm", [128, 128], F32)
        rowm = sb("rowm", [128, 4], F32)
        qf = sb("qf", [128, T], F32)
        bA = sb("bA", [128, T], F32)
        kk = sb("kk", [128, T], F32)
        cg = sb("cg", [128, T], F32)
        bD = sb("bD", [128, T], F32)
        ex = sb("ex", [128, 2, T], F32)
        qtil = sb("qtil", [128, 2, T], BF16)
        qbar = sb("qbar", [128, 2, T], BF16)
        qhat = sb("qhat", [128, 1, T], BF16)
        ktil = sb("ktil", [128, 2, T], BF16)
        khatT = sb("khatT", [128, 2, T], BF16)
        khm = sb("khm", [128, 2, 8, 4, 128], BF16)
        vtok = sb("vtok", [128, 8, 2, 128], BF16)
        attm = sb("attm", [128, 2, 128], BF16)
        Sm = sb("Sm", [128, 2, 2, 128], F32)
        Sbf = sb("Sbf", [128, 2, 8, 128], BF16)
        Dc = sb("Dc", [128, 2, NCHK], F32)
        ost = sb("ost", [128, 2, T], F32)

        t_c = P.dma("sp", bdm[:], aps["bdmask"], waits=W0, inc=csem)
        t_c = P.dma("sp", rowm[:], aps["rowmask"], waits=W0, inc=csem)
        P.op("dve", lambda: nc.vector.memset(segm[:], 1.0), waits=W0)
        P.op("dve", lambda: nc.vector.memset(segm[:].rearrange("p (c l) -> p c l", l=CL)[:, :, 0:1], 0.0))
        P.op("dve", lambda: nc.vector.memset(onec[:], 1.0))
        t_ms = P.op("dve", lambda: nc.vector.memset(zeroc[:], 0.0), inc=dve_d)

        def slab_load(j):
            return C.ws13.load(lambda t, sl: t[:, sl], Winr[j])

        def proj_fm(slw, t_w, hh, evac):
            s, fr = pss.next()
            t_pe = None
            for k in range(KT):
                for bi, (n0, nn) in enumerate(nts):
                    lastmm = (k == KT - 1 and bi == 1)
                    t_pe = P.op("pe", lambda k=k, bi=bi, n0=n0, nn=nn, s=s: nc.tensor.matmul(
                        pss.ps[:, 4 * s + bi, 0:nn], lhsT=w13[:, slw, k, hh * 128:(hh + 1) * 128],
                        rhs=xnT[:, k, n0:n0 + nn], start=(k == 0), stop=(k == KT - 1)),
                        waits=[t_w, fr, t_norm], inc=pe_u if lastmm else None)
            psv = pss.ps[:, 4 * s:4 * s + 2, :].rearrange("p b n -> p (b n)")
            t_rel = evac(psv, t_pe)
            pss.release(s, t_rel)
            return t_pe

        st = {"qf": None, "bA": None, "kk": None, "cg": None, "bD": None, "ex": [None, None], "exi": 0,
              "khatT": [None, None], "vtok": None, "ost": [None, None], "osem": [P.sem(name + "os0"), P.sem(name + "os1")],
              "qh": [None, None], "qsem": [P.sem(name + "qs0"), P.sem(name + "qs1")], "ssem": [P.sem(name + "ss0"), P.sem(name + "ss1")],
              "Sm": [None, None], "tilfree": [None, None], "khm": [None, None], "attm": [None, None]}
        finals = []
        cg3 = cg[:].rearrange("p (c l) -> p c l", l=CL)

        def next_ex():
            i = st["exi"] % 2
            st["exi"] += 1
            return i

        for sp in range(16):
            slq, t_wq = slab_load(sp)
            slz, t_wz = slab_load(16 + sp)
            sli, t_wi = slab_load(32 + sp)
            per_head = []
            chain = []
            for hh in range(2):
                h = 2 * sp + hh
                def ev_q(psv, t_pe):
                    t = P.op("act", lambda: nc.scalar.activation(out=qf[:], in_=psv, func=AF.Copy), waits=[t_pe, st["qf"]], inc=act_d)
                    return t
                t_pq = proj_fm(slq, t_wq, hh, ev_q)
                t_qf = (act_d, act_d.n)
                def ev_z(psv, t_pe):
                    return P.op("act", lambda: nc.scalar.activation(out=bA[:], in_=psv, func=AF.Sigmoid), waits=[t_pe, st["bA"]], inc=act_d)
                t_pz = proj_fm(slz, t_wz, hh, ev_z)
                if hh == 1:
                    C.ws13.release(slq, t_pz)
                    C.ws13.release(slz, t_pz)
                t_sg = (act_d, act_d.n)
                t_f = P.op("dve", lambda h=h: nc.vector.tensor_scalar(out=bA[:], in0=bA[:], scalar1=H.oml[:, h:h + 1], scalar2=H.lbc[:, h:h + 1],
                                                                    op0=ALU.mult, op1=ALU.add), waits=[t_sg, H.t], inc=dve_d)
                t_kk = P.op("dve", lambda: nc.vector.tensor_scalar(out=kk[:], in0=bA[:], scalar1=-1.0, scalar2=1.0, op0=ALU.mult, op1=ALU.add),
                            waits=[t_f, st["kk"]], inc=dve_d)
                t_eg = P.op("dve", lambda: nc.vector.tensor_tensor_scan(out=bD[:], data0=bA[:], data1=zeroc[:, 0:1].to_broadcast([128, T]),
                                                                       initial=1.0, op0=ALU.mult, op1=ALU.add),
                            waits=[t_kk, t_ms, st["bD"]], inc=dve_d)
                t_qh = P.op("dve", lambda: nc.vector.tensor_tensor(out=qhat[:, 0, :], in0=qf[:], in1=bD[:], op=ALU.mult),
                            waits=[t_eg, t_qf, st["qh"][0]], inc=dve_d)
                st["bD"] = t_qh
                t_qst = P.dma("sp", scr["qhat"][h], qhat[:, 0, :], waits=[t_qh], inc=st["qsem"][0])
                st["qh"][0] = t_qst
                finals.append(t_qst)
                t_ln = P.op("act", lambda: nc.scalar.activation(out=bA[:], in_=bA[:], func=AF.Ln), waits=[t_eg], inc=act_d)
                t_cg = P.op("dve", lambda: nc.vector.tensor_tensor_scan(out=cg[:], data0=segm[:], data1=bA[:], initial=0.0,
                                                                       op0=ALU.mult, op1=ALU.add), waits=[t_ln, st["cg"]], inc=dve_d)
                st["bA"] = t_cg
                t_dc = P.op("act", lambda hh=hh: nc.scalar.activation(out=Dc[:, hh, :], in_=cg3[:, :, CL - 1], func=AF.Exp),
                            waits=[t_cg, st["Sm"][hh]], inc=act_d)
                e0 = next_ex()
                t_e = P.op("act", lambda e0=e0: nc.scalar.activation(out=ex[:, e0, :], in_=cg[:], func=AF.Exp), waits=[t_cg, st["ex"][e0]], inc=act_d)
                t_qb = P.op("dve", lambda e0=e0, hh=hh: nc.vector.tensor_tensor(out=qbar[:, hh, :], in0=qf[:], in1=ex[:, e0, :], op=ALU.mult),
                            waits=[t_e, st["tilfree"][hh]], inc=dve_d)
                st["ex"][e0] = t_qb
                t_d1 = P.op("dve", lambda: nc.vector.tensor_tensor(out=bA[:].rearrange("p (c l) -> p c l", l=CL), in0=cg3,
                                                                  in1=cg3[:, :, CL // 2 - 1:CL // 2].to_broadcast([128, NCHK, CL]), op=ALU.subtract),
                            waits=[t_cg], inc=dve_d)
                e1 = next_ex()
                t_e = P.op("act", lambda e1=e1: nc.scalar.activation(out=ex[:, e1, :], in_=bA[:], func=AF.Exp), waits=[t_d1, st["ex"][e1]], inc=act_d)
                t_qt = P.op("dve", lambda e1=e1, hh=hh: nc.vector.tensor_tensor(out=qtil[:, hh, :], in0=qf[:], in1=ex[:, e1, :], op=ALU.mult),
                            waits=[t_e], inc=dve_d)
                st["ex"][e1] = t_qt
                st["qf"] = t_qt
                e2 = next_ex()
                t_e = P.op("act", lambda e2=e2: nc.scalar.activation(out=ex[:, e2, :], in_=bA[:], func=AF.Exp, scale=-1.0),
                           waits=[t_d1, st["ex"][e2]], inc=act_d)
                t_kt = P.op("dve", lambda e2=e2, hh=hh: nc.vector.tensor_tensor(out=ktil[:, hh, :], in0=kk[:], in1=ex[:, e2, :], op=ALU.mult),
                            waits=[t_e], inc=dve_d)
                st["ex"][e2] = t_kt
                t_d2 = P.op("dve", lambda: nc.vector.tensor_tensor(out=bA[:].rearrange("p (c l) -> p c l", l=CL),
                                                                  in0=cg3[:, :, CL - 1:CL].to_broadcast([128, NCHK, CL]), in1=cg3, op=ALU.subtract),
                            waits=[t_e], inc=dve_d)
                st["cg"] = t_d2
                e3 = next_ex()
                t_e = P.op("act", lambda e3=e3: nc.scalar.activation(out=ex[:, e3, :], in_=bA[:], func=AF.Exp), waits=[t_d2, st["ex"][e3]], inc=act_d)
                st["bA"] = t_e
                t_kh = P.op("dve", lambda e3=e3, hh=hh: nc.vector.tensor_tensor(out=khatT[:, hh, :], in0=kk[:], in1=ex[:, e3, :], op=ALU.mult),
                            waits=[t_e, st["khatT"][hh]], inc=dve_d)
                st["ex"][e3] = t_kh
                st["kk"] = t_kh
                chain.append(dict(h=h, hh=hh, t_qb=t_qb, t_qt=t_qt, t_kt=t_kt, t_kh=t_kh, t_dc=t_dc))
            t_vt = None
            for half in range(2):
                s, fr = pss.next()
                t_pe = None
                for bi in range(4):
                    tt = half * 4 + bi
                    for k in range(KT):
                        lastmm = (bi == 3 and k == KT - 1)
                        t_pe = P.op("pe", lambda k=k, bi=bi, tt=tt, s=s, sli=sli: nc.tensor.matmul(
                            pss.ps[:, 4 * s + bi, 0:256], lhsT=xnT[:, k, tt * 128:(tt + 1) * 128], rhs=w13[:, sli, k, :],
                            start=(k == 0), stop=(k == KT - 1)),
                            waits=[t_wi, fr, t_norm], inc=pe_u if lastmm else None)
                if half == 1:
                    C.ws13.release(sli, t_pe)
                t_vt = P.op("act", lambda s=s, half=half: nc.scalar.activation(
                    out=vtok[:, half * 4:half * 4 + 4, :, :].rearrange("p t h e -> p t (h e)"),
                    in_=pss.ps[:, 4 * s:4 * s + 4, 0:256], func=AF.Copy),
                    waits=[t_pe, st["vtok"]], inc=act_d)
                pss.release(s, t_vt)
            for ch in chain:
                h, hh, t_qb, t_qt, t_kt, t_kh, t_dc = ch['h'], ch['hh'], ch['t_qb'], ch['t_qt'], ch['t_kt'], ch['t_kh'], ch['t_dc']
                s, fr = pss.next()
                t_pe = None
                for tt in range(8):
                    t_pe = P.op("pe", lambda tt=tt, s=s, hh=hh: nc.tensor.transpose(out=psb[:, 4 * s, tt * 128:(tt + 1) * 128],
                                                                             in_=khatT[:, hh, tt * 128:(tt + 1) * 128], identity=C.identb[:, :]),
                                waits=[t_kh, fr], inc=pe_u if tt == 7 else None)
                st["khatT"][hh] = t_pe
                t_km = None
                for c4 in range(4):
                    t_km = P.op("act", lambda c4=c4, s=s, hh=hh: nc.scalar.activation(
                        out=khm[:, hh, :, c4, :], in_=psb[:, 4 * s, :].rearrange("p (t d) -> p t d", t=8), func=AF.Copy,
                        scale=rowm[:, c4:c4 + 1]), waits=[t_pe, t_c, st["khm"][hh]], inc=act_d)
                pss.release(s, t_km)
                t_s0 = P.op("dve", lambda hh=hh: nc.vector.memset(Sm[:, hh, 0, :], 0.0), waits=[st["Sm"][hh]], inc=dve_d)
                per_head.append(dict(h=h, hh=hh, t_ops=[t_qb, t_qt, t_kt, t_km, t_vt], t_dc=t_dc, t_s0=t_s0))
            lastS = [None, None]
            cp_hist = [[None, None], [None, None]]
            last_rd = [None, None]
            t_oev = [None, None]
            for tt in range(8):
                ph1 = []
                for hd in per_head:
                    hh = hd["hh"]
                    s, fr = pss.next()
                    t_at = P.op("pe", lambda s=s, hh=hh, tt=tt: nc.tensor.matmul(
                        pss.ps[:, 4 * s, 0:128], lhsT=ktil[:, hh, tt * 128:(tt + 1) * 128], rhs=qtil[:, hh, tt * 128:(tt + 1) * 128],
                        start=True, stop=True), waits=hd["t_ops"] + [fr], inc=pe_u)
                    t_kv = None
                    for c4 in range(4):
                        t_kv = P.op("pe", lambda s=s, hh=hh, tt=tt, c4=c4: nc.tensor.matmul(
                            pss.ps[:, 4 * s + 1, c4 * 128:(c4 + 1) * 128], lhsT=khm[:, hh, tt, c4, :], rhs=vtok[:, tt, hh, :],
                            start=True, stop=True), inc=pe_u if c4 == 3 else None)
                    t_am = P.op("dve", lambda s=s, hh=hh: nc.vector.tensor_tensor(out=attm[:, hh, :], in0=pss.ps[:, 4 * s, 0:128], in1=bdm[:],
                                                                             op=ALU.mult), waits=[t_at, t_c, st["attm"][hh]], inc=dve_d)
                    ph1.append((hd, s, t_am, t_kv))
                for hd, s, t_am, t_kv in ph1:
                    hh = hd["hh"]
                    t_oi = P.op("pe", lambda s=s, hh=hh, tt=tt: nc.tensor.matmul(
                        pss.ps[:, 4 * s + 2, 0:128], lhsT=vtok[:, tt, hh, :], rhs=attm[:, hh, :], start=True, stop=False),
                        waits=[t_am], inc=pe_u)
                    st["attm"][hh] = t_oi
                    t_in = t_oi
                    for c4 in range(4):
                        c = tt * 4 + c4
                        slot = c % 8
                        if c > 0:
                            t_in = P.op("pe", lambda s=s, hh=hh, tt=tt, c4=c4, slot=slot: nc.tensor.matmul(
                                pss.ps[:, 4 * s + 2, c4 * CL:(c4 + 1) * CL], lhsT=Sbf[:, hh, slot, :],
                                rhs=qbar[:, hh, tt * 128 + c4 * CL:tt * 128 + (c4 + 1) * CL], start=False, stop=(c4 == 3)),
                                waits=[lastS[hh]], inc=pe_u)
                        t_up = P.op("dve", lambda s=s, hh=hh, c=c, c4=c4: nc.vector.scalar_tensor_tensor(
                            out=Sm[:, hh, (c + 1) % 2, :], in0=Sm[:, hh, c % 2, :], scalar=Dc[:, hh, c:c + 1],
                            in1=pss.ps[:, 4 * s + 1, c4 * 128:(c4 + 1) * 128],
                            op0=ALU.mult, op1=ALU.add), waits=[t_kv, hd["t_dc"], hd["t_s0"], cp_hist[hh][0]], inc=dve_d)
                        nslot = (c + 1) % 8
                        lastS[hh] = P.op("act", lambda hh=hh, nslot=nslot, c=c: nc.scalar.activation(out=Sbf[:, hh, nslot, :], in_=Sm[:, hh, (c + 1) % 2, :],
                                                                                                func=AF.Copy),
                                         waits=[t_up, last_rd[hh]], inc=act_d)
                        cp_hist[hh] = [cp_hist[hh][1], lastS[hh]]
                        t_lastup = t_up
                    last_rd[hh] = t_in
                    t_oev[hh] = P.op("act", lambda s=s, hh=hh, tt=tt: nc.scalar.activation(out=ost[:, hh, tt * 128:(tt + 1) * 128],
                                                                                      in_=pss.ps[:, 4 * s + 2, 0:128], func=AF.Copy),
                                     waits=[t_in, st["ost"][hh]], inc=act_d)
                    pss.release(s, t_oev[hh])
                    hd["t_lastup"] = t_lastup
            for hd in per_head:
                hh = hd["hh"]
                h = hd["h"]
                t_os = P.dma("sp", scr["oloc"][h], ost[:, hh, :], waits=[t_oev[hh]], inc=st["osem"][hh])
                st["ost"][hh] = t_os
                t_ss = P.dma("sp", scr["sfin"][h], Sm[:, hh, 0, :], waits=[hd["t_lastup"]], inc=st["ssem"][hh])
                st["Sm"][hh] = t_ss
                st["tilfree"][hh] = last_rd[hh]
                st["khm"][hh] = last_rd[hh]
                finals += [t_os, t_ss]
            st["vtok"] = last_rd[1]
    return finals


def hgrn_pass2(P, C, x, row0, aps, scr, sprev, ready, name, renorm_src=None):
    from contextlib import ExitStack
    nc = P.nc
    pss = C.pss
    xnT = C.xnT
    w13 = C.w13
    T = 1024
    nts = ntiles_of(T)
    Winr = aps["c_w_in"]
    Wout = aps["c_w_out"]
    with ExitStack() as ctx:
        pe_u = P.sem(name + "peu")
        act_d = P.sem(name + "actd")
        dve_d = P.sem(name + "dved")
        csem = P.sem(name + "csem")
        W0 = list(ready)
        if renorm_src is not None:
            t_norm = rmsnorm_T(P, ctx, pss, renorm_src, T, aps["mix_norm"], C.ident, xnT, ready, name + "n")
            W0 = [t_norm]

        def sb(nm, shape, dt):
            return ctx.enter_context(nc.sbuf_tensor(name + nm, shape, dt))
        ogc = sb("ogc", [128, 1], F32)
        outT = sb("outT", [128, 16, T], BF16)
        flagc = sb("flagc", [128, 1], F32)
        t_c = P.dma("sp", ogc[:], aps["ogain"], waits=W0, inc=csem)
        t_c = P.dma("sp", flagc[:], aps["flag"], waits=W0, inc=csem)
        st_tick = {}
        fin = None
        t_prev_part = W0
        for part in range(2):
            with ExitStack() as pctx:
                def sbp(nm, shape, dt):
                    return pctx.enter_context(nc.sbuf_tensor(name + nm + str(part), shape, dt))
                of = sbp("of", [128, 2, T], F32)
                qh = sbp("qh", [128, 2, T], BF16)
                s32 = sbp("s32", [128, 2, 128], F32)
                sbf = sbp("sbf", [128, 2, 128], BF16)
                sgt = sbp("sgt", [128, T], F32)
                sqf = sbp("sqf", [128, T], F32)
                ldo = [P.shared(name + f"lo{i}") for i in range(2)]
                ldq = [P.shared(name + f"lq{i}") for i in range(2)]
                lds = [P.shared(name + f"ls{i}") for i in range(2)]
                fr_of = [None, None]
                fr_qh = [None, None]
                fr_s32 = [None, None]
                fr_sbf = [None, None]
                fr_sgt = None
                fr_sqf = None
                t_last = None
                for hl in range(16):
                    h = part * 16 + hl
                    b = hl % 2
                    if hl % 2 == 0:
                        slg, t_wg = C.ws13.load(lambda t, sl: t[:, sl], Winr[48 + h // 2])
                    hh = h % 2
                    t_lo = P.dma("sp", of[:, b, :], scr["oloc"][h], waits=[fr_of[b]] + list(t_prev_part), inc=ldo[b])
                    t_lq = P.dma("sp", qh[:, b, :], scr["qhat"][h], waits=[fr_qh[b]] + list(t_prev_part), inc=ldq[b])
                    t_ls = P.dma("sp", s32[:, b, :], sprev[h], waits=[fr_s32[b]] + list(t_prev_part), inc=lds[b])
                    t_sb = P.op("dve", lambda b=b: nc.vector.tensor_scalar(out=sbf[:, b, :], in0=s32[:, b, :], scalar1=flagc[:, 0:1], scalar2=None,
                                                                          op0=ALU.mult), waits=[t_ls, fr_sbf[b], t_c], inc=dve_d)
                    fr_s32[b] = t_sb
                    s, fr = pss.next()
                    t_pe = None
                    for k in range(KT):
                        for bi, (n0, nn) in enumerate(nts):
                            lastmm = (k == KT - 1 and bi == 1)
                            t_pe = P.op("pe", lambda k=k, bi=bi, n0=n0, nn=nn, s=s, slg=slg, hh=hh: nc.tensor.matmul(
                                pss.ps[:, 4 * s + bi, 0:nn], lhsT=w13[:, slg, k, hh * 128:(hh + 1) * 128],
                                rhs=xnT[:, k, n0:n0 + nn], start=(k == 0), stop=(k == KT - 1)),
                                waits=[t_wg, fr] + W0, inc=pe_u if lastmm else None)
                    if hh == 1:
                        C.ws13.release(slg, t_pe)
                    t_sg = P.op("act", lambda s=s: nc.scalar.activation(out=sgt[:], in_=pss.ps[:, 4 * s:4 * s + 2, :].rearrange("p b n -> p (b n)"),
                                                                        func=AF.Silu), waits=[t_pe, fr_sgt] + list(t_prev_part), inc=act_d)
                    pss.release(s, t_sg)
                    s, fr = pss.next()
                    t_pc = None
                    for bi, (n0, nn) in enumerate(nts):
                        t_pc = P.op("pe", lambda bi=bi, n0=n0, nn=nn, s=s, b=b: nc.tensor.matmul(
                            pss.ps[:, 4 * s + bi, 0:nn], lhsT=sbf[:, b, :], rhs=qh[:, b, n0:n0 + nn], start=True, stop=True),
                            waits=[t_sb, t_lq, fr], inc=pe_u if bi == 1 else None)
                    fr_qh[b] = t_pc
                    fr_sbf[b] = t_pc
                    t_o = P.op("dve", lambda s=s, b=b: nc.vector.tensor_tensor(
                        out=of[:, b, :], in0=pss.ps[:, 4 * s:4 * s + 2, :].rearrange("p b n -> p (b n)"), in1=of[:, b, :], op=ALU.add),
                        waits=[t_pc, t_lo], inc=dve_d)
                    pss.release(s, t_o)
                    t_sq = P.op("act", lambda b=b: nc.scalar.activation(out=sqf[:], in_=of[:, b, :], func=AF.Square), waits=[t_o, fr_sqf], inc=act_d)
                    s2, fr2 = pss.next()
                    t_p2 = None
                    for bi, (n0, nn) in enumerate(nts):
                        t_p2 = P.op("pe", lambda bi=bi, n0=n0, nn=nn, s2=s2: nc.tensor.matmul(
                            pss.ps[:, 4 * s2 + bi, 0:nn], lhsT=C.ones128[:, :], rhs=sqf[:, n0:n0 + nn], start=True, stop=True),
                            waits=[t_sq, fr2, C.t_ident], inc=pe_u if bi == 1 else None)
                    t_rt = P.op("act", lambda s2=s2: nc.scalar.activation(out=sqf[:], in_=pss.ps[:, 4 * s2:4 * s2 + 2, :].rearrange("p b n -> p (b n)"),
                                                                          func=AF.Sqrt, scale=1.0 / 128, bias=C.epsc[:, 0:1]), waits=[t_p2], inc=act_d)
                    pss.release(s2, t_rt)
                    t_rc = P.op("dve", lambda: nc.vector.reciprocal(out=sqf[:], in_=sqf[:]), waits=[t_rt], inc=dve_d)
                    t_m1 = P.op("dve", lambda b=b: nc.vector.scalar_tensor_tensor(out=of[:, b, :], in0=of[:, b, :], scalar=ogc[:, 0:1], in1=sqf[:],
                                                                                op0=ALU.mult, op1=ALU.mult), waits=[t_rc, t_c], inc=dve_d)
                    fr_sqf = t_m1
                    t_last = P.op("dve", lambda b=b, hl=hl: nc.vector.tensor_tensor(out=outT[:, hl, :], in0=of[:, b, :], in1=sgt[:], op=ALU.mult),
                                  waits=[t_m1, t_sg], inc=dve_d)
                    fr_of[b] = t_last
                    fr_sgt = t_last
            with ExitStack() as octx:
                def load_wo(n, part=part):
                    src = Wout[part][n]
                    sl, t = C.ws13.load(lambda t_, sl_: t_[:, sl_].rearrange("p k f -> p (k f)").rearrange("p (k f) -> p k f", k=16), src)
                    wv = w13[:, sl].rearrange("p k f -> p (k f)").rearrange("p (k f) -> p k f", k=16)
                    return (lambda c: wv[:, c, :]), t, (lambda tk, sl=sl: C.ws13.release(sl, tk))
                fin = accum_rows(P, C, name + f"o{part}", outT, 16, load_wo, x, row0, 1.0, [t_last], st_tick, dve_d, pe_u, octx)
                t_prev_part = [fin[-1], fin[-2], fin[-3]]
    return fin


N_CORES = 8
CONST_SHAPES = {"ident": [128, 128], "mk_cur": [128, 128], "mk_prev": [128, 128], "mk_prev0": [128, 128], "sinks_l": [128, 16],
                "qg": [64, 1], "kg": [64, 1], "ogain": [128, 1], "bdmask": [128, 128], "rowmask": [128, 4], "flag": [128, 1]}


def weight_shapes(ff):
    npr, nch = ff // 256, ff // 128
    return {"ffn1_norm": [2, D], "ffn1_w1": [2, npr, 128, KT, 256], "ffn1_w3": [2, npr, 128, KT, 256], "ffn1_w2": [2, 8, 128, nch, 512],
            "mix_norm": [2, D],
            "ffn2_norm": [2, D], "ffn2_w1": [2, npr, 128, KT, 256], "ffn2_w3": [2, npr, 128, KT, 256], "ffn2_w2": [2, 8, 128, nch, 512],
            "ab_w_in": [26, 128, KT, 256], "ab_v_ln_g": [1, 2048], "ab_v_ln_b": [1, 2048], "ab_w_s": [1, 16, 128, 128], "ab_b_s": [1, 16, 128],
            "ab_w_out": [2, 8, 128, 16, 512], "c_w_in": [64, 128, KT, 256], "c_lb_logits": [2, D], "c_w_out": [2, 8, 128, 16, 512]}


def tile_in(w):
    lead = w.shape[:-2]
    F = w.shape[-1]
    v = w.reshape(*lead, KT, 128, F // 256, 256)
    nl = len(lead)
    return np.ascontiguousarray(np.transpose(v, (*range(nl), nl + 2, nl + 1, nl, nl + 3)))


def tile_w2(w):
    lead = w.shape[:-2]
    R = w.shape[-2]
    v = w.reshape(*lead, R // 128, 128, 8, 512)
    nl = len(lead)
    return np.ascontiguousarray(np.transpose(v, (*range(nl), nl + 2, nl + 1, nl, nl + 3)))


def tile_out(w):
    v = w.reshape(2, 16, 128, 8, 512)
    return np.ascontiguousarray(np.transpose(v, (0, 3, 2, 1, 4)))


def build_program(ff=FF, n_cores=N_CORES):
    from contextlib import ExitStack
    nc = bass.Bass("TRN2", target_bir_lowering=False)
    xin = nc.dram_tensor("xin", [1024, D], F32, kind="ExternalInput").ap()
    w = {n: nc.dram_tensor(n, s, F32, kind="ExternalInput").ap() for n, s in weight_shapes(ff).items()}
    cst = {n: nc.dram_tensor(n, s, F32, kind="ExternalInput").ap() for n, s in CONST_SHAPES.items()}
    out = nc.dram_tensor("out", [1024, D], F32, kind="ExternalOutput").ap()
    xres = nc.dram_tensor("xres", [1024, D], F32, kind="Internal").ap()
    h_out = nc.dram_tensor("halo_out", [192, 512], BF16, kind="Internal").ap()
    h_all = nc.dram_tensor("halo_all", [384, 512], BF16, kind="Internal", addr_space="Local").ap()
    oloc = nc.dram_tensor("oloc", [32, 128, 1024], F32, kind="Internal").ap()
    qhat = nc.dram_tensor("qhat", [32, 128, 1024], BF16, kind="Internal").ap()
    sfin = nc.dram_tensor("sfin", [4096, 128], F32, kind="Internal").ap()
    sall = nc.dram_tensor("sall", [8192, 128], F32, kind="Internal", addr_space="Local").ap()
    nch = ff // 128
    P = Prog(nc)
    with ExitStack() as st:
        C = make_ctx(P, st, cst["ident"])
        xo = xres
        groups = [[2 * i, 2 * i + 1] for i in range(n_cores // 2)]
        f = ffn_stage(P, C, xin, xres, 1024, w["ffn1_norm"][0], w["ffn1_w1"][0], w["ffn1_w3"][0], w["ffn1_w2"][0], [C.t_ident], "fa0", nch=nch)
        ab_aps = {"mix_norm": w["mix_norm"][0], "ab_w_in": w["ab_w_in"], "ab_w_out": w["ab_w_out"], "ab_v_ln_g": w["ab_v_ln_g"][0],
                  "ab_v_ln_b": w["ab_v_ln_b"][0], "ab_w_s": w["ab_w_s"][0], "ab_b_s": w["ab_b_s"][0]}
        for k in ("qg", "kg", "sinks_l", "mk_cur", "mk_prev", "mk_prev0"):
            ab_aps[k] = cst[k]
        f = ab_stage(P, C, xres, ab_aps, f, "ab", (h_out, h_all), groups)
        f = ffn_stage(P, C, xo, xo, 1024, w["ffn2_norm"][0], w["ffn2_w1"][0], w["ffn2_w3"][0], w["ffn2_w2"][0], f, "fb0", nch=nch)
        f = ffn_stage(P, C, xo, xo, 1024, w["ffn1_norm"][1], w["ffn1_w1"][1], w["ffn1_w3"][1], w["ffn1_w2"][1], f, "fa1", nch=nch)
        h_aps = {"mix_norm": w["mix_norm"][1], "c_w_in": w["c_w_in"], "c_lb_logits": w["c_lb_logits"], "c_w_out": w["c_w_out"],
                 "ogain": cst["ogain"], "bdmask": cst["bdmask"], "rowmask": cst["rowmask"], "flag": cst["flag"]}
        scr = {"oloc": oloc, "qhat": qhat, "sfin": sfin.rearrange("(h d) e -> h d e", d=128)}
        f1 = hgrn_pass1(P, C, xo, h_aps, scr, f, "h1")
        ccsem = P.sem("cc")
        t_cc = P.op("pool", lambda: nc.gpsimd.collective_compute("AllGather", ALU.bypass, replica_groups=groups,
                                                                 ins=[sfin[:, :]], outs=[sall[:, :]]), waits=f1, inc=ccsem)
        sprev = sall[0:4096, :].rearrange("(h d) e -> h d e", d=128)
        f = hgrn_pass2(P, C, xres, 0, h_aps, scr, sprev, list(f1) + [t_cc], "h2")
        f = ffn_stage(P, C, xo, out, 1024, w["ffn2_norm"][1], w["ffn2_w1"][1], w["ffn2_w3"][1], w["ffn2_w2"][1], f, "fb1", nch=nch)
        P.wait("sp", f)
        P.emit()
    return nc


def host_constants():
    j = np.arange(128)[:, None]
    t = np.arange(128)[None, :]
    c = {"ident": np.eye(128, dtype=np.float32),
         "mk_cur": (j <= t).astype(np.float32),
         "mk_prev": (j > t).astype(np.float32),
         "bdmask": ((j // CL == t // CL) & (j <= t)).astype(np.float32),
         "rowmask": (np.arange(128)[:, None] // 32 == np.arange(4)[None, :]).astype(np.float32)}
    return c


def make_in_maps(inputs, n_cores=N_CORES):
    x = np.asarray(inputs["x"], dtype=np.float32)
    hc = host_constants()
    shared = {}
    for k, v in inputs.items():
        if k in ("x", "ab_q_norm", "ab_k_norm", "ab_sinks", "c_o_norm"):
            continue
        v = np.asarray(v, dtype=np.float32)
        if k in ("ffn1_w1", "ffn1_w3", "ffn2_w1", "ffn2_w3"):
            v = tile_in(v)
        elif k in ("ffn1_w2", "ffn2_w2"):
            v = tile_w2(v)
        elif k in ("ab_w_in", "c_w_in"):
            v = tile_in(v[0])
        elif k in ("ab_w_out", "c_w_out"):
            v = tile_out(v[0])
        shared[k] = np.ascontiguousarray(v)
    shared["qg"] = np.asarray(inputs["ab_q_norm"], np.float32).reshape(64, 1)
    shared["kg"] = np.asarray(inputs["ab_k_norm"], np.float32).reshape(64, 1)
    shared["ogain"] = np.asarray(inputs["c_o_norm"], np.float32).reshape(128, 1)
    shared["sinks_l"] = np.ascontiguousarray(np.repeat(np.asarray(inputs["ab_sinks"], np.float32).reshape(16, 2), 64, axis=1).T)
    for k in ("ident", "mk_cur", "mk_prev", "bdmask", "rowmask"):
        shared[k] = hc[k]
    maps = []
    for c in range(n_cores):
        b, half = c // 2, c % 2
        xin = np.ascontiguousarray(x[b, half * 1024:(half + 1) * 1024])
        m = dict(shared)
        m["xin"] = xin
        m["mk_prev0"] = hc["mk_prev"] if half == 1 else np.zeros((128, 128), np.float32)
        m["flag"] = np.full((128, 1), float(half), np.float32)
        maps.append(m)
    return maps


_PROG = {}


def kernel(**inputs):
    ff = int(np.asarray(inputs["ffn1_w1"]).shape[-1])
    n_cores = 2 * int(np.asarray(inputs["x"]).shape[0])
    key = (ff, n_cores)
    if key not in _PROG:
        _PROG[key] = build_program(ff, n_cores)
    nc = _PROG[key]
    maps = make_in_maps(inputs, n_cores)
    res = run_bass_kernel_spmd(nc, maps, core_ids=list(range(n_cores)))
    B = n_cores // 2
    out = np.empty((B, 2048, D), np.float32)
    for c in range(n_cores):
        out[c // 2, (c % 2) * 1024:(c % 2 + 1) * 1024] = res.results[c]["out"]
    return out
```
